# Optimizing a Trainium2 kernel written in Bass

```python
import math
import jax
import jax.numpy as jnp
from jax import lax
import numpy as np

D_MODEL = 1024
BATCH = 2
SEQ = 8192
DEPTH = 1

N_HEADS = 8
HEAD_DIM = 64
ATTN_WIDTH = N_HEADS * HEAD_DIM
MOBA_BLOCK = 256
MOBA_TOPK = 3
MOBA_Q_CHUNK = 64
POOL_WINDOWS = (2, 4, 8, 16)
POOL_GROUPS = len(POOL_WINDOWS)
POOL_WIDTH = D_MODEL // 2
POOL_GROUP_WIDTH = POOL_WIDTH // POOL_GROUPS
N_BRANCHES = 2
IN_WIDTH = 3 * ATTN_WIDTH + POOL_WIDTH + N_BRANCHES * D_MODEL
REL_BUCKETS = 32
REL_MAX_DIST = 128
N_EXPERT_GROUPS = 4
EXPERTS_PER_GROUP = 8
N_EXPERTS = N_EXPERT_GROUPS * EXPERTS_PER_GROUP
EXPERT_TOPK = 2
D_EXPERT = D_MODEL // 2
N_MOD = 6
EPS = 1e-6
NEG_INF = -1e30

kernel_name = "hybrid_moba_pool_hmoe_adaln_block"


def rms_norm(x, g):
    xf = x.astype(jnp.float32)
    xf = xf * lax.rsqrt(jnp.mean(xf * xf, axis=-1, keepdims=True) + EPS)
    return (xf * g.astype(jnp.float32)).astype(x.dtype)


def t5_bucket(rel):
    n = jnp.maximum(rel, 0)
    max_exact = REL_BUCKETS // 2
    nf = jnp.maximum(n, 1).astype(jnp.float32)
    large = max_exact + (jnp.log(nf / max_exact) / math.log(REL_MAX_DIST / max_exact)
                         * (REL_BUCKETS - max_exact)).astype(jnp.int32)
    large = jnp.minimum(large, REL_BUCKETS - 1)
    return jnp.where(n < max_exact, n, large)


def moba_attention(q, k, v, rel_bias):
    B, S, H, Dh = q.shape
    nb = -(-S // MOBA_BLOCK)
    pad = nb * MOBA_BLOCK - S
    topk = min(MOBA_TOPK, nb)
    scale = Dh ** -0.5
    qh = q.transpose(0, 2, 1, 3)
    kp = jnp.pad(k.transpose(0, 2, 1, 3), ((0, 0), (0, 0), (0, pad), (0, 0)))
    vp = jnp.pad(v.transpose(0, 2, 1, 3), ((0, 0), (0, 0), (0, pad), (0, 0)))
    kb = kp.reshape(B, H, nb, MOBA_BLOCK, Dh)
    vb = vp.reshape(B, H, nb, MOBA_BLOCK, Dh)
    k_mean = jnp.mean(kb.astype(jnp.float32), axis=3)
    bias_t = rel_bias.T
    b_idx = jnp.arange(B)[:, None, None, None]
    h_idx = jnp.arange(H)[None, :, None, None]
    h_idx5 = jnp.arange(H)[None, :, None, None, None]
    blk_offsets = jnp.arange(MOBA_BLOCK)

    def attend_chunk(ci):
        q0 = ci * MOBA_Q_CHUNK
        blk = q0 // MOBA_BLOCK
        qc = lax.dynamic_slice_in_dim(qh, q0, MOBA_Q_CHUNK, axis=2)
        qpos = q0 + jnp.arange(MOBA_Q_CHUNK)
        gate = jnp.einsum('bhqd,bhnd->bhqn', qc.astype(jnp.float32), k_mean)
        gate = jnp.where(jnp.arange(nb) < blk, gate, -jnp.inf)
        _, sel = lax.top_k(gate, topk)
        sel_valid = sel < blk
        ks = kb[b_idx, h_idx, sel]
        vs = vb[b_idx, h_idx, sel]
        s_sel = jnp.einsum('bhqd,bhqjkd->bhqjk', qc, ks).astype(jnp.float32) * scale
        kpos_sel = sel[..., None] * MOBA_BLOCK + blk_offsets
        bucket_sel = t5_bucket(qpos[None, None, :, None, None] - kpos_sel)
        s_sel = s_sel + bias_t[h_idx5, bucket_sel].astype(jnp.float32)
        s_sel = jnp.where(sel_valid[..., None], s_sel, NEG_INF)
        ko = lax.dynamic_index_in_dim(kb, blk, axis=2, keepdims=False)
        vo = lax.dynamic_index_in_dim(vb, blk, axis=2, keepdims=False)
        s_own = jnp.einsum('bhqd,bhkd->bhqk', qc, ko).astype(jnp.float32) * scale
        rel_own = qpos[:, None] - (blk * MOBA_BLOCK + blk_offsets)[None, :]
        s_own = s_own + bias_t[:, t5_bucket(rel_own)][None].astype(jnp.float32)
        s_own = jnp.where(rel_own >= 0, s_own, NEG_INF)
        logits = jnp.concatenate(
            [s_sel.reshape(B, H, MOBA_Q_CHUNK, topk * MOBA_BLOCK), s_own], axis=-1)
        p = jax.nn.softmax(logits, axis=-1).astype(v.dtype)
        p_sel = p[..., :topk * MOBA_BLOCK].reshape(B, H, MOBA_Q_CHUNK, topk, MOBA_BLOCK)
        p_own = p[..., topk * MOBA_BLOCK:]
        return (jnp.einsum('bhqjk,bhqjkd->bhqd', p_sel, vs)
                + jnp.einsum('bhqk,bhkd->bhqd', p_own, vo))

    out = lax.map(attend_chunk, jnp.arange(S // MOBA_Q_CHUNK))
    return out.transpose(1, 0, 3, 2, 4).reshape(B, S, H * Dh)


def pool_mixer(u, pool_w, pool_scale):
    B, S, P = u.shape
    uf = u.astype(jnp.float32)
    cs = jnp.pad(jnp.cumsum(uf, axis=1), ((0, 0), (1, 0), (0, 0)))
    t = jnp.arange(S)
    outs = []
    for g, w in enumerate(POOL_WINDOWS):
        lo_c, hi_c = g * POOL_GROUP_WIDTH, (g + 1) * POOL_GROUP_WIDTH
        csg = cs[..., lo_c:hi_c]
        lo = jnp.maximum(t + 1 - w, 0)
        cnt = (t + 1 - lo).astype(jnp.float32)
        mean = (csg[:, 1:] - csg[:, lo]) / cnt[None, :, None]
        d = (mean - uf[..., lo_c:hi_c]).astype(u.dtype)
        outs.append(jnp.einsum('bsc,cd->bsd', d, pool_w[g]))
    return jnp.concatenate(outs, axis=-1) * pool_scale


def hier_moe(h, w_rg, b_rg, w_re, b_re, w_g, w_u, w_d):
    B, S, D = h.shape
    N = B * S
    hf = h.reshape(N, D)
    z_group = (hf @ w_rg).astype(jnp.float32) + b_rg.astype(jnp.float32)
    p_group = jax.nn.softmax(z_group, axis=-1)
    pg_top, g_idx = lax.top_k(p_group, 1)
    z_exp = ((hf @ w_re).astype(jnp.float32) + b_re.astype(jnp.float32)).reshape(
        N, N_EXPERT_GROUPS, EXPERTS_PER_GROUP)
    z_in_group = z_exp[jnp.arange(N), g_idx[:, 0]]
    ze_top, e_idx = lax.top_k(z_in_group, EXPERT_TOPK)
    weights = pg_top * jax.nn.softmax(ze_top, axis=-1)
    expert_id = g_idx * EXPERTS_PER_GROUP + e_idx
    combine = jnp.einsum('nk,nke->ne', weights,
                         jax.nn.one_hot(expert_id, N_EXPERTS, dtype=jnp.float32)).astype(h.dtype)
    y = jnp.zeros_like(hf)
    for e in range(N_EXPERTS):
        act = jax.nn.silu(hf @ w_g[e]) * (hf @ w_u[e])
        y = y + combine[:, e:e + 1] * (act @ w_d[e])
    return y.reshape(B, S, D)


def setup_inputs(seed: int = 0) -> dict:
    key = jax.random.key(seed)
    ks = jax.random.split(key, 22)
    f32 = jnp.float32
    D = D_MODEL
    PGW = POOL_GROUP_WIDTH

    def nrm(k, shape, s):
        return jax.random.normal(k, shape, f32) * s

    return {
        "x": nrm(ks[0], (BATCH, SEQ, D), 1.0),
        "c": nrm(ks[1], (BATCH, D), 1.0),
        "w_ada": nrm(ks[2], (DEPTH, D, N_MOD * D), 0.5 * D ** -0.5),
        "b_ada": nrm(ks[3], (DEPTH, N_MOD * D), 0.02),
        "norm1_g": 1.0 + nrm(ks[4], (DEPTH, D), 0.05),
        "w_in": nrm(ks[5], (DEPTH, D, IN_WIDTH), D ** -0.5),
        "b_gate": nrm(ks[6], (DEPTH, N_BRANCHES * D), 0.02),
        "rel_bias": nrm(ks[7], (REL_BUCKETS, N_HEADS), 0.5),
        "pool_w": nrm(ks[8], (DEPTH, POOL_GROUPS, PGW, PGW), PGW ** -0.5),
        "pool_scale": 1.0 + nrm(ks[9], (DEPTH, POOL_WIDTH), 0.1),
        "w_branch_attn": nrm(ks[10], (DEPTH, ATTN_WIDTH, D), ATTN_WIDTH ** -0.5),
        "w_branch_pool": nrm(ks[11], (DEPTH, POOL_WIDTH, D), POOL_WIDTH ** -0.5),
        "w_out": nrm(ks[12], (DEPTH, D, D), D ** -0.5),
        "norm2_g": 1.0 + nrm(ks[13], (DEPTH, D), 0.05),
        "w_router_group": nrm(ks[14], (DEPTH, D, N_EXPERT_GROUPS), D ** -0.5),
        "b_router_group": nrm(ks[15], (DEPTH, N_EXPERT_GROUPS), 0.01),
        "w_router_expert": nrm(ks[16], (DEPTH, D, N_EXPERTS), D ** -0.5),
        "b_router_expert": nrm(ks[17], (DEPTH, N_EXPERTS), 0.01),
        "w_expert_gate": nrm(ks[18], (DEPTH, N_EXPERTS, D, D_EXPERT), D ** -0.5),
        "w_expert_up": nrm(ks[19], (DEPTH, N_EXPERTS, D, D_EXPERT), D ** -0.5),
        "w_expert_down": nrm(ks[20], (DEPTH, N_EXPERTS, D_EXPERT, D), D_EXPERT ** -0.5),
        "norm_f_g": 1.0 + nrm(ks[21], (D,), 0.05),
    }


def reference(x, c, w_ada, b_ada, norm1_g, w_in, b_gate, rel_bias, pool_w, pool_scale,
              w_branch_attn, w_branch_pool, w_out, norm2_g, w_router_group, b_router_group,
              w_router_expert, b_router_expert, w_expert_gate, w_expert_up, w_expert_down,
              norm_f_g):
    B, S, D = x.shape
    c_act = jax.nn.silu(c)
    split_pts = [ATTN_WIDTH, 2 * ATTN_WIDTH, 3 * ATTN_WIDTH, 3 * ATTN_WIDTH + POOL_WIDTH]
    for l in range(DEPTH):
        mod = (c_act @ w_ada[l] + b_ada[l])[:, None, :]
        shift1, scale1, gate1, shift2, scale2, gate2 = jnp.split(mod, N_MOD, axis=-1)
        h = rms_norm(x, norm1_g[l]) * (1 + scale1) + shift1
        proj = h @ w_in[l]
        q, k, v, u, z_gate = jnp.split(proj, split_pts, axis=-1)
        q = q.reshape(B, S, N_HEADS, HEAD_DIM)
        k = k.reshape(B, S, N_HEADS, HEAD_DIM)
        v = v.reshape(B, S, N_HEADS, HEAD_DIM)
        y_attn = moba_attention(q, k, v, rel_bias) @ w_branch_attn[l]
        y_pool = pool_mixer(u, pool_w[l], pool_scale[l]) @ w_branch_pool[l]
        g_attn, g_pool = jnp.split(jax.nn.sigmoid(z_gate + b_gate[l]), N_BRANCHES, axis=-1)
        mixed = (g_attn * y_attn + g_pool * y_pool) @ w_out[l]
        x = x + gate1 * mixed
        h2 = rms_norm(x, norm2_g[l]) * (1 + scale2) + shift2
        y_moe = hier_moe(h2, w_router_group[l], b_router_group[l], w_router_expert[l],
                         b_router_expert[l], w_expert_gate[l], w_expert_up[l], w_expert_down[l])
        x = x + gate2 * y_moe
    return rms_norm(x, norm_f_g)
```

```python
import math
from contextlib import ExitStack
import numpy as np
import concourse.bass as bass
import concourse.mybir as mybir
from concourse.bass_utils import run_bass_kernel_spmd

F32 = mybir.dt.float32
BF16 = mybir.dt.bfloat16
AF = mybir.ActivationFunctionType
ALU = mybir.AluOpType
AX = mybir.AxisListType

D = 1024
S = 8192
NOWN = 2048
NH = 8
NEG = -30000.0
NEXP = 32


class Buf:
    __slots__ = ("name", "w", "r", "excl")

    def __init__(self, name="", excl=False):
        self.name = name
        self.w = None
        self.r = {}
        self.excl = excl


class Eng:
    def __init__(self, name, h, sem):
        self.name, self.h, self.sem, self.cnt, self.seen = name, h, sem, 0, {}


class Ctx:
    def __init__(self, nc, stack, n_dma_slots=8):
        self.nc = nc
        self.E = {}
        for name, h in (("pe", nc.tensor), ("act", nc.scalar), ("dve", nc.vector),
                        ("pool", nc.gpsimd), ("sp", nc.sync)):
            self.E[name] = Eng(name, h, stack.enter_context(nc.semaphore("sem_" + name)))
        self.dma_slots = {}
        for q in ("sp", "pool"):
            sl = [[stack.enter_context(nc.semaphore(f"dq_{q}_{i}")), 0] for i in range(n_dma_slots)]
            self.dma_slots[q] = [sl, 0]
        self.out_events = []

    def _wait(self, eng, ev):
        if ev is None:
            return
        key, sem, val = ev
        if key == eng.name and eng.name == "pe":
            return
        if eng.seen.get(key, 0) >= val:
            return
        eng.seen[key] = val
        eng.h.wait_ge(sem, val)

    @staticmethod
    def _split(reads, writes):
        return list(reads), list(writes)

    def _deps(self, eng, reads, writes):
        for b in reads:
            self._wait(eng, b.w)
            if b.excl:
                for key, (sem, val) in list(b.r.items()):
                    if key != eng.name:
                        self._wait(eng, (key, sem, val))
        for b in writes:
            self._wait(eng, b.w)
            for key, (sem, val) in list(b.r.items()):
                self._wait(eng, (key, sem, val))

    def _commit(self, ev, reads, writes):
        key, sem, val = ev
        for b in reads:
            b.r[key] = (sem, val)
        for b in writes:
            b.w = ev
            b.r = {}

    def op(self, en, fn, reads=(), writes=()):
        eng = self.E[en]
        reads, writes = self._split(reads, writes)
        self._deps(eng, reads, writes)
        eng.cnt += 1
        fn(eng.h).then_inc(eng.sem, 1)
        self._commit((eng.name, eng.sem, eng.cnt), reads, writes)

    def group(self, en, fns, reads=(), writes=()):
        eng = self.E[en]
        reads, writes = self._split(reads, writes)
        self._deps(eng, reads, writes)
        for f in fns[:-1]:
            f(eng.h)
        eng.cnt += 1
        fns[-1](eng.h).then_inc(eng.sem, 1)
        self._commit((eng.name, eng.sem, eng.cnt), reads, writes)

    def dma(self, q, out, in_, reads=(), writes=(), is_output=False, **kw):
        eng = self.E[q]
        slots, idx = self.dma_slots[q]
        k = idx % len(slots)
        slot = slots[k]
        self.dma_slots[q][1] = idx + 1
        key = f"dq_{q}_{k}"
        if slot[1] > 0:
            self._wait(eng, (key, slot[0], slot[1]))
        self._deps(eng, reads, writes)
        slot[1] += 16
        eng.h.dma_start(out=out, in_=in_, **kw).then_inc(slot[0], 16)
        ev = (key, slot[0], slot[1])
        self._commit(ev, reads, writes)
        if is_output:
            self.out_events.append(ev)

    def barrier(self):
        evs = [(e.name, e.sem, e.cnt) for e in self.E.values() if e.cnt > 0]
        for q, (slots, idx) in self.dma_slots.items():
            for i, sl in enumerate(slots):
                if sl[1] > 0:
                    evs.append((f"dq_{q}_{i}", sl[0], sl[1]))
        for e in self.E.values():
            for ev in evs:
                self._wait(e, ev)

    def finish(self):
        eng = self.E["sp"]
        for ev in self.out_events:
            self._wait(eng, ev)
        for q, (slots, idx) in self.dma_slots.items():
            for i, sl in enumerate(slots):
                if sl[1] > 0:
                    self._wait(eng, (f"dq_{q}_{i}", sl[0], sl[1]))


class Arena:
    def __init__(self, ap, nbytes):
        self.ap = ap
        self.free = [(0, nbytes)]
        self.live = {}

    def alloc(self, name, shape, dt):
        esz = 4 if dt == F32 else 2
        n = esz
        for s in shape[1:]:
            n *= s
        n = (n + 63) // 64 * 64
        for i, (o, sz) in enumerate(self.free):
            if sz >= n:
                self.free[i] = (o + n, sz - n)
                self.live[name] = (o, n)
                v = self.ap[0:shape[0], o // 2:(o + n) // 2]
                if dt == F32:
                    v = v.bitcast(F32)
                nel = 1
                for s in shape[1:]:
                    nel *= s
                v = v[:, 0:nel]
                if len(shape) == 3:
                    v = v.rearrange("p (a b) -> p a b", a=shape[1])
                elif len(shape) == 4:
                    v = v.rearrange("p (a b c) -> p a b c", a=shape[1], b=shape[2])
                return v
        raise RuntimeError(f"arena OOM for {name} {shape} free={self.free}")

    def release(self, *names):
        for name in names:
            o, n = self.live.pop(name)
            self.free.append((o, n))
        self.free.sort()
        m = []
        for o, n in self.free:
            if n == 0:
                continue
            if m and m[-1][0] + m[-1][1] == o:
                m[-1] = (m[-1][0], m[-1][1] + n)
            else:
                m.append((o, n))
        self.free = m


def build_program(stop=None, debug=False, opts=None):
    opts = opts or {}
    nc = bass.Bass("TRN2", target_bir_lowering=False)
    skind = "ExternalOutput" if debug else "Internal"

    def din(name, shape, dt=F32):
        return nc.dram_tensor(name, list(shape), dt, kind="ExternalInput").ap()

    x_all = din("x_all", [S, D])
    x_own = din("x_own", [2560, D])
    w_in = din("w_in", [D, 4096])
    w_ada = din("w_ada", [D, 6144])
    b_ada = din("b_ada", [1, 6144])
    ccol = din("ccol", [128, 8])
    g1col = din("g1col", [128, 8])
    g2col = din("g2col", [128, 8])
    gf_rep_d = din("gf_rep", [128, D])
    bgate_col = din("bgate_col", [128, 16])
    pscale_col = din("pscale_col", [128, 4])
    b31_rep_d = din("b31_rep", [128, 8])
    pool_w = din("pool_w", [4, 128, 128])
    w_ba = din("w_ba", [512, D])
    w_bp = din("w_bp", [512, D])
    w_out = din("w_out", [D, D])
    w_r = din("w_r", [D, 36])
    b_r_rep_d = din("b_r_rep", [128, 36])
    w_eg = din("w_eg", [NEXP, D, 512])
    w_eu = din("w_eu", [NEXP, D, 512])
    w_ed = din("w_ed", [NEXP, 512, D])
    bias_near = din("bias_near", [NH, 128, 10, 256])
    e_rows = din("e_rows", [32, S])
    negmask_d = din("negmask", [128, 16, 32])
    valid_d = din("valid01", [128, 16, 32])
    ownm1_d = din("ownm1", [128, 16, 32])
    halo_d = din("halo_mask", [128, 16])
    invc_d = din("invcnt0", [128, 4, 64])
    ident_d = din("ident", [128, 128])
    out_d = nc.dram_tensor("out", [NOWN, D], F32, kind="ExternalOutput").ap()
    kt_dram = nc.dram_tensor("kt_scr", [512, S], BF16, kind=skind).ap()
    v_dram = nc.dram_tensor("v_scr", [S, 520], BF16, kind=skind).ap()
    x1_dram = nc.dram_tensor("x1_scr", [NOWN, D], F32, kind=skind).ap()

    with ExitStack() as st:
        ARENA_BYTES = 211968
        arena_t = st.enter_context(nc.sbuf_tensor("arena", [128, ARENA_BYTES // 2], BF16))
        psum_t = st.enter_context(nc.psum_tensor("psum", [128, 4096], F32))
        C = Ctx(nc, st)
        A = Arena(arena_t, ARENA_BYTES)

        def bank(k, dt=F32):
            v = psum_t[:, k * 512:(k + 1) * 512]
            return v.bitcast(BF16) if dt == BF16 else v

        PB = [Buf(f"bank{k}", excl=opts.get("excl", True)) for k in range(8)]

        def dump(name, ap, bufs):
            if not debug:
                return
            d = nc.dram_tensor("dbg_" + name, list(ap.shape), ap.dtype, kind="ExternalOutput").ap()
            C.dma("sp", d, ap, reads=bufs, is_output=True)

        def stop_here(tag):
            if stop == tag:
                C.finish()
                return True
            return False

        def wload(dst, src_rows_ap, kcn, bufs):
            C.dma("pool", dst, src_rows_ap.rearrange("(k p) n -> p k n", p=128), writes=bufs)

        ident_f = A.alloc("ident_f", [128, 128], F32)
        ident_b = A.alloc("ident_b", [128, 128], BF16)
        cc = A.alloc("cc", [128, 8], F32)
        g1c = A.alloc("g1c", [128, 8], F32)
        g2c = A.alloc("g2c", [128, 8], F32)
        gf_rep = A.alloc("gf_rep", [128, D], F32)
        bgc = A.alloc("bgc", [128, 16], F32)
        psc = A.alloc("psc", [128, 4], F32)
        b31 = A.alloc("b31", [128, 8], F32)
        b_r_rep = A.alloc("b_r_rep", [128, 36], F32)
        negmask = A.alloc("negmask", [128, 16, 32], F32)
        valid01 = A.alloc("valid01", [128, 16, 32], F32)
        ownm1 = A.alloc("ownm1", [128, 16, 32], F32)
        halo = A.alloc("halo", [128, 16], F32)
        invc = A.alloc("invc", [128, 4, 64], F32)
        zero_c = A.alloc("zero_c", [128, 1], F32)
        eps_c = A.alloc("eps_c", [128, 1], F32)
        ones_r = A.alloc("ones_r", [1, 128], F32)
        modcol = A.alloc("modcol", [128, 4, 8], F32)
        gm1 = A.alloc("gm1", [128, 8], F32)
        gm2 = A.alloc("gm2", [128, 8], F32)
        gate1_rep = A.alloc("gate1_rep", [128, D], F32)
        gate2_rep = A.alloc("gate2_rep", [128, D], F32)
        kmean8 = A.alloc("kmean8", [64, 8, 32], F32)
        comb = A.alloc("comb", [128, 16, 32], F32)
        KC = Buf("consts")
        CB = Buf("comb")
        for dst, src in ((ident_f, ident_d), (cc, ccol), (g1c, g1col), (g2c, g2col), (gf_rep, gf_rep_d),
                         (bgc, bgate_col), (psc, pscale_col), (b31, b31_rep_d), (b_r_rep, b_r_rep_d),
                         (negmask, negmask_d), (valid01, valid_d), (ownm1, ownm1_d), (halo, halo_d),
                         (invc, invc_d)):
            C.dma("sp", dst, src, writes=[KC])
        C.dma("pool", ident_b, ident_d, writes=[KC])
        C.op("dve", lambda e: e.memset(zero_c, 0.0), writes=[KC])
        C.op("dve", lambda e: e.memset(eps_c, 1e-6), writes=[KC])
        C.op("dve", lambda e: e.memset(ones_r, 1.0), writes=[KC])

        dump("negmask", negmask, [KC]); dump("ident_b", ident_b, [KC])
        if stop_here("00"):
            return nc
        cact = A.alloc("cact", [128, 8], F32)
        mod_row = A.alloc("mod_row", [1, 6144], F32)
        bada = A.alloc("bada", [1, 6144], F32)
        wst = [A.alloc(f"wada_st{i}", [128, 8, 512], F32) for i in range(2)]
        WST = [Buf(), Buf()]
        BM = Buf("mod")
        C.dma("sp", bada, b_ada, writes=[BM])
        C.op("act", lambda e: e.activation(out=cact, in_=cc, func=AF.Silu), reads=[KC], writes=[BM])
        def ada_cols(cts):
            for ct in cts:
                s_ = ct % 2
                C.dma("sp", wst[s_], w_ada[:, ct * 512:(ct + 1) * 512].rearrange("(k p) n -> p k n", p=128),
                      writes=[WST[s_]])
                pb = bank(ct % 2)
                C.group("pe", [(lambda e, kc=kc, s_=s_, pb=pb: e.matmul(pb[0:1, :], cact[:, kc:kc + 1], wst[s_][:, kc, :],
                                                                         start=(kc == 0), stop=(kc == 7)))
                               for kc in range(8)], reads=[BM, WST[s_]], writes=[PB[ct % 2]])
                C.op("dve", lambda e, ct=ct, pb=pb: e.tensor_tensor(out=mod_row[:, ct * 512:(ct + 1) * 512], in0=pb[0:1, :],
                                                                     in1=bada[:, ct * 512:(ct + 1) * 512], op=ALU.add),
                     reads=[PB[ct % 2], BM], writes=[BM])

        ada_cols(range(0, 4))
        mod_dram = nc.dram_tensor("mod_scr", [1, 6144], F32, kind=skind).ap()
        MD = Buf()
        C.dma("sp", mod_dram[:, 0:2048], mod_row[:, 0:2048], reads=[BM], writes=[MD])
        for vi, v in ((0, 0), (1, 1)):
            C.dma("sp", modcol[:, vi, :], mod_dram[0, v * 1024:(v + 1) * 1024].rearrange("(k p) -> p k", p=128),
                  reads=[MD], writes=[BM], allow_slow_non_contiguous=True)
        C.op("dve", lambda e: e.scalar_tensor_tensor(out=gm1, in0=modcol[:, 1, :], scalar=1.0, in1=g1c, op0=ALU.add, op1=ALU.mult),
             reads=[BM, KC], writes=[BM])
        sh1 = modcol[:, 0, :]
        sh2 = modcol[:, 2, :]

        def ada_finish():
            MD2 = Buf()
            C.dma("sp", mod_dram[:, 2048:6144], mod_row[:, 2048:6144], reads=[BM], writes=[MD2])
            for vi, v in ((2, 3), (3, 4)):
                C.dma("sp", modcol[:, vi, :], mod_dram[0, v * 1024:(v + 1) * 1024].rearrange("(k p) -> p k", p=128),
                      reads=[MD2], writes=[BM2], allow_slow_non_contiguous=True)
            C.op("dve", lambda e: e.scalar_tensor_tensor(out=gm2, in0=modcol[:, 3, :], scalar=1.0, in1=g2c, op0=ALU.add, op1=ALU.mult),
                 reads=[BM2, KC], writes=[BM2])
            for grep, v in ((gate1_rep, 2), (gate2_rep, 5)):
                C.dma("sp", grep, mod_dram[0:1, v * 1024:(v + 1) * 1024].to_broadcast([128, 1024]), reads=[MD2], writes=[BM2])
            dump("modcol", modcol, [BM, BM2]); dump("gm1", gm1, [BM]); dump("gate1_rep", gate1_rep, [BM2]); dump("gate2_rep", gate2_rep, [BM2])

        BM2 = Buf("mod2")
        if stop == "0":
            ada_cols(range(4, 12))
            ada_finish()
            C.barrier()
            C.finish()
            return nc

        xa = [A.alloc(f"xa{i}", [128, D], F32) for i in range(3)]
        XA = [Buf() for _ in range(3)]
        xs = [A.alloc(f"xs{i}", [128, D], BF16) for i in range(6)]
        XS = [Buf() for _ in range(6)]
        junk = A.alloc("junk", [128, D], BF16)
        JK = Buf()
        stat = A.alloc("stat", [128, 8, 4], F32)
        ST = [Buf() for _ in range(8)]
        ncount = [0]

        def norm_s1(xin, XIN, slot):
            i = ncount[0]
            ncount[0] += 1
            s4 = stat[:, i % 8, :]
            SB_ = ST[i % 8]
            xsb, XSB = xs[slot], XS[slot]
            C.op("act", lambda e: e.activation(out=junk, in_=xin, func=AF.Square, accum_out=s4[:, 0:1]),
                 reads=[XIN], writes=[SB_])
            C.op("act", lambda e: e.activation(out=s4[:, 1:2], in_=s4[:, 0:1], func=AF.Sqrt, scale=1.0 / D, bias=eps_c),
                 reads=[SB_, KC], writes=[SB_])
            C.op("dve", lambda e: e.reciprocal(out=s4[:, 2:3], in_=s4[:, 1:2]), reads=[SB_], writes=[SB_])
            C.op("dve", lambda e: e.tensor_scalar(out=xsb, in0=xin, scalar1=s4[:, 2:3], scalar2=None, op0=ALU.mult),
                 reads=[XIN, SB_], writes=[XSB])

        def norm_s2(slot, gm, sh, out3, OUT, tpbank):
            xsb, XSB = xs[slot], XS[slot]
            tp = bank(tpbank, BF16).rearrange("p (a b) -> p a b", a=8)
            C.group("pe", [(lambda e, kc=kc: e.transpose(tp[:, kc, :], xsb[:, kc * 128:(kc + 1) * 128], ident_b))
                           for kc in range(8)], reads=[XSB, KC], writes=[PB[tpbank]])
            for kc in range(8):
                C.op("act", lambda e, kc=kc: e.activation(out=out3[:, kc, :], in_=tp[:, kc, :], func=AF.Identity,
                                                          scale=gm[:, kc:kc + 1], bias=sh[:, kc:kc + 1]),
                     reads=[PB[tpbank], BM], writes=[OUT[kc]])

        def norm_T(xin, XIN, gm, sh, out3, OUT, tpbank):
            slot = ncount[0] % len(xs)
            norm_s1(xin, XIN, slot)
            norm_s2(slot, gm, sh, out3, OUT, tpbank)

        wkv = A.alloc("wkv", [128, 8, 1024], BF16)
        WKV = Buf()
        wload(wkv, w_in[:, 512:1536], 8, [WKV])
        hT = [A.alloc(f"hT{i}", [128, 8, 512], BF16) for i in range(2)]
        HT = [Buf() for _ in range(2)]
        ktsb = [A.alloc(f"ktsb{i}", [128, 512], BF16) for i in range(2)]
        KTS = [Buf() for _ in range(2)]
        vsb = [A.alloc(f"vsb{i}", [128, 8, 65], BF16) for i in range(2)]
        VSB = [Buf() for _ in range(2)]
        kmsum = A.alloc("kmsum", [128, 4, 32], F32)
        KMS = Buf()
        KTD, VD = Buf("ktd"), Buf("vd")
        for i in range(2):
            C.op("dve", lambda e, i=i: e.memset(vsb[i], 1.0), writes=[VSB[i]])
        HTK = [[Buf() for _ in range(8)] for _ in range(2)]

        def a_s1(tile):
            C.dma("sp", xa[tile % 3], x_all[tile * 128:(tile + 1) * 128, :], writes=[XA[tile % 3]])
            norm_s1(xa[tile % 3], XA[tile % 3], tile % 6)

        def a_s2(tile):
            tg_, t_ = tile // 4, tile % 4
            norm_s2(tile % 6, gm1, sh1, hT[tg_ % 2][:, :, t_ * 128:(t_ + 1) * 128], HTK[tg_ % 2], tile % 2)

        for tile in range(4):
            a_s1(tile)
        for tile in range(4):
            a_s2(tile)
        kcnt = 0
        kbanks = (2, 3, 6)
        vbanks = (4, 5, 7)
        for tg in range(16):
            hs, HS = hT[tg % 2], HTK[tg % 2]
            if tg < 15:
                for t in range(4):
                    a_s1((tg + 1) * 4 + t)
            if tg == 1:
                ada_cols(range(4, 12))
            for hp in range(4):
                bk = kbanks[kcnt % 3]
                pk = bank(bk)
                C.group("pe", [(lambda e, kc=kc, hp=hp, pk=pk, hs=hs: e.matmul(
                    pk, wkv[:, kc, hp * 128:(hp + 1) * 128], hs[:, kc, :], start=(kc == 0), stop=(kc == 7)))
                    for kc in range(8)], reads=[WKV] + HS, writes=[PB[bk]])
                ks, KS = ktsb[kcnt % 2], KTS[kcnt % 2]
                C.op("dve", lambda e, ks=ks, pk=pk: e.tensor_copy(out=ks, in_=pk), reads=[PB[bk]], writes=[KS])
                C.op("dve", lambda e, ks=ks, hp=hp, tg=tg: e.tensor_reduce(
                    out=kmsum[:, hp, 2 * tg:2 * tg + 2], in_=ks.rearrange("p (a b) -> p a b", a=2), axis=AX.X, op=ALU.add),
                    reads=[KS], writes=[KMS])
                C.dma("sp", kt_dram[hp * 128:(hp + 1) * 128, tg * 512:(tg + 1) * 512], ks, reads=[KS], writes=[KTD])
                kcnt += 1
                if tg < 15:
                    a_s2((tg + 1) * 4 + hp)
            for t in range(4):
                tile = tg * 4 + t
                bk = vbanks[tile % 3]
                pv = bank(bk)
                C.group("pe", [(lambda e, kc=kc, t=t, pv=pv, hs=hs: e.matmul(
                    pv, hs[:, kc, t * 128:(t + 1) * 128], wkv[:, kc, 512:1024], start=(kc == 0), stop=(kc == 7)))
                    for kc in range(8)], reads=[WKV] + HS, writes=[PB[bk]])
                vs_, VS_ = vsb[tile % 2], VSB[tile % 2]
                C.op("dve", lambda e, vs_=vs_, pv=pv: e.tensor_copy(
                    out=vs_[:, :, 0:64], in_=pv.rearrange("p (a b) -> p a b", a=8)), reads=[PB[bk]], writes=[VS_])
                C.dma("sp", v_dram[tile * 128:(tile + 1) * 128, :], vs_.rearrange("p a b -> p (a b)"),
                      reads=[VS_], writes=[VD])
        for hp in range(4):
            for hh in range(2):
                C.dma("sp", kmean8[:, 2 * hp + hh, :], kmsum[hh * 64:(hh + 1) * 64, hp, :], reads=[KMS], writes=[KMS])
        ada_finish()
        dump("kmean8", kmean8, [KMS])
        C.barrier()
        if stop_here("A"):
            return nc
        A.release("cact", "mod_row", "bada", "wada_st0", "wada_st1")
        A.release("wkv", "hT0", "hT1", "ktsb0", "ktsb1", "vsb0", "vsb1", "kmsum")

        h_own = A.alloc("h_own", [128, 8, 32, 80], BF16)
        h_own_f = h_own.rearrange("p k c t -> p k (c t)")
        HOK = [Buf() for _ in range(8)]
        qaug = A.alloc("qaug", [96, 8, NOWN], BF16)
        QA = [Buf() for _ in range(8)]
        pmT = A.alloc("pmT", [128, 4, NOWN], BF16)
        PM = Buf()
        wq = A.alloc("wq", [128, 8, 512], BF16)
        wu = A.alloc("wu", [128, 8, 512], BF16)
        pwb = A.alloc("pwb", [128, 4, 128], BF16)
        WQ, WU, PW = Buf(), Buf(), Buf()
        wload(wq, w_in[:, 0:512], 8, [WQ])
        wload(wu, w_in[:, 1536:2048], 8, [WU])
        C.dma("pool", pwb, pool_w.rearrange("g c d -> c g d"), writes=[PW])
        for tile in range(20):
            C.dma("sp", xa[tile % 3], x_own[tile * 128:(tile + 1) * 128, :], writes=[XA[tile % 3]])
            norm_T(xa[tile % 3], XA[tile % 3], gm1, sh1, h_own_f[:, :, tile * 128:(tile + 1) * 128], HOK, tile % 2)
        qtf = [A.alloc(f"qtf{i}", [64, NOWN], F32) for i in range(2)]
        QTF = [Buf() for _ in range(2)]
        mbT = [A.alloc(f"mbT{i}", [32, NOWN], BF16) for i in range(2)]
        MBT = [Buf() for _ in range(2)]
        gA = A.alloc("gA", [128, 16, 32], F32)
        gB = A.alloc("gB", [128, 16, 32], F32)
        gMk = A.alloc("gMk", [128, 16, 32], F32)
        gTb2 = [A.alloc(f"gTb{i}", [128, 16, 32], BF16) for i in range(2)]
        GTB2 = [Buf(), Buf()]
        gmx = A.alloc("gmx", [128, 3, 16], F32)
        GA, GB, GMK, GMX = Buf(), Buf(), Buf(), Buf()
        ptb2 = psum_t[:, 6 * 512:8 * 512].bitcast(BF16)
        qn = 0
        def b_part1(h):
            nonlocal qn
            qf, QF = qtf[h % 2], QTF[h % 2]
            for tg in range(4):
                bk = 2 + (qn % 2)
                pq = bank(bk)
                qn += 1
                C.group("pe", [(lambda e, kc=kc, h=h, tg=tg, pq=pq: e.matmul(
                    pq[0:64, :], wq[:, kc, h * 64:(h + 1) * 64], h_own[:, kc, tg * 8:(tg + 1) * 8, 16:80],
                    start=(kc == 0), stop=(kc == 7))) for kc in range(8)], reads=[WQ] + HOK, writes=[PB[bk]])
                C.op("act", lambda e, h=h, tg=tg, pq=pq: e.activation(
                    out=qaug[0:64, h, tg * 512:(tg + 1) * 512], in_=pq[0:64, :], func=AF.Copy, scale=0.125),
                    reads=[PB[bk]], writes=[QA[h]])
                C.op("dve", lambda e, qf=qf, pq=pq, tg=tg: e.tensor_copy(out=qf[:, tg * 512:(tg + 1) * 512], in_=pq[0:64, :]),
                     reads=[PB[bk]], writes=[QF])
            bg = 4 + (h % 2)
            pg = bank(bg).rearrange("p (a b) -> p a b", a=16)
            C.group("pe", [(lambda e, qt=qt, pg=pg, qf=qf, h=h: e.matmul(
                pg[:, qt, :], qf[:, qt * 128:(qt + 1) * 128], kmean8[:, h, :], start=True, stop=True)) for qt in range(16)],
                reads=[QF, KMS], writes=[PB[bg]])
            C.op("dve", lambda e, pg=pg: e.tensor_tensor(out=gA, in0=pg, in1=negmask, op=ALU.add), reads=[PB[bg], KC], writes=[GA])
            cur, CUR, oth, OTH = gA, GA, gB, GB
            for rnd in range(3):
                C.op("dve", lambda e, cur=cur, rnd=rnd: e.tensor_reduce(out=gmx[:, rnd, :], in_=cur, axis=AX.X, op=ALU.max),
                     reads=[CUR], writes=[GMX])
                if rnd == 2:
                    break
                C.op("dve", lambda e, cur=cur, rnd=rnd: e.tensor_tensor(
                    out=gMk, in0=cur, in1=gmx[:, rnd, :].unsqueeze(2).to_broadcast([128, 16, 32]), op=ALU.is_ge),
                    reads=[CUR, GMX], writes=[GMK])
                C.op("dve", lambda e, cur=cur, oth=oth: e.scalar_tensor_tensor(
                    out=oth, in0=gMk, scalar=-1e30, in1=cur, op0=ALU.mult, op1=ALU.add), reads=[GMK, CUR], writes=[OTH])
                cur, CUR, oth, OTH = oth, OTH, cur, CUR
            C.op("dve", lambda e, pg=pg: e.tensor_tensor(out=gB, in0=pg, in1=negmask, op=ALU.add), reads=[PB[bg], KC], writes=[GB])
            C.op("dve", lambda e: e.tensor_tensor(out=gMk, in0=gB, in1=gmx[:, 2, :].unsqueeze(2).to_broadcast([128, 16, 32]), op=ALU.is_ge),
                 reads=[GB, GMX], writes=[GMK])
            C.op("dve", lambda e: e.tensor_tensor(out=gMk, in0=gMk, in1=valid01, op=ALU.mult), reads=[GMK, KC], writes=[GMK])
            C.op("dve", lambda e, h=h: e.tensor_tensor(out=gTb2[h % 2], in0=gMk, in1=ownm1, op=ALU.add), reads=[GMK, KC], writes=[GTB2[h % 2]])

        def b_part2(h):
            C.group("pe", [(lambda e, qt=qt: e.transpose(ptb2[0:32, qt * 128:(qt + 1) * 128], gTb2[h % 2][:, qt, :], ident_b))
                           for qt in range(16)], reads=[GTB2[h % 2], KC], writes=[PB[6], PB[7]])
            for hb in range(2):
                C.op("act", lambda e, hb=hb, h=h: e.activation(
                    out=mbT[h % 2][:, hb * 1024:(hb + 1) * 1024], in_=ptb2[0:32, hb * 1024:(hb + 1) * 1024], func=AF.Copy,
                    scale=-NEG), reads=[PB[6 + hb]], writes=[MBT[h % 2]])
            C.dma("sp", qaug[64:96, h, :], mbT[h % 2], reads=[MBT[h % 2]], writes=[QA[h]])
        b_part1(0)
        for h in range(NH):
            if h + 1 < NH:
                b_part1(h + 1)
            b_part2(h)
        C.barrier()
        A.release("qtf0", "qtf1", "mbT0", "mbT1", "gA", "gB", "gMk", "gTb0", "gTb1", "gmx", "wq")
        uT = A.alloc("uT", [128, 32, 80], F32)
        uT_f = uT.rearrange("p c t -> p (c t)")
        sA = A.alloc("sA", [128, 32, 80], F32)
        sB = A.alloc("sB", [128, 32, 80], F32)
        dT = A.alloc("dT", [128, 32, 64], BF16)
        dT_f = dT.rearrange("p c t -> p (c t)")
        tmp64 = A.alloc("tmp64", [128, 64], F32)
        UT, SA_, SB2, DT_, T64 = Buf(), Buf(), Buf(), Buf(), Buf()
        pn = 0
        for g in range(4):
            for cg in range(5):
                bk = 2 + (pn % 2)
                pu = bank(bk)
                C.group("pe", [(lambda e, kc=kc, g=g, cg=cg, pu=pu: e.matmul(
                    pu, wu[:, kc, g * 128:(g + 1) * 128], h_own_f[:, kc, cg * 512:(cg + 1) * 512],
                    start=(kc == 0), stop=(kc == 7))) for kc in range(8)], reads=[WU] + HOK, writes=[PB[bk]])
                C.op("act", lambda e, cg=cg, pu=pu: e.activation(out=uT_f[:, cg * 512:(cg + 1) * 512], in_=pu, func=AF.Copy),
                     reads=[PB[bk]], writes=[UT])
                pn += 1
            C.op("dve", lambda e: e.tensor_tensor(out=uT_f[:, 0:16], in0=uT_f[:, 0:16], in1=halo, op=ALU.mult),
                 reads=[UT, KC], writes=[UT])
            cur, CUR = uT, UT
            nxt = [(sA, SA_), (sB, SB2)]
            sh_ = 1
            for k in range(g + 1):
                dst, DST = nxt[k % 2]
                lo = 2 * sh_ - 1
                C.op("pool", lambda e, dst=dst, cur=cur, lo=lo, sh_=sh_: e.tensor_tensor(
                    out=dst[:, :, lo:80], in0=cur[:, :, lo:80], in1=cur[:, :, lo - sh_:80 - sh_], op=ALU.add),
                    reads=[CUR], writes=[DST])
                cur, CUR = dst, DST
                sh_ *= 2
            w = 2 ** (g + 1)
            C.op("dve", lambda e, cur=cur, w=w: e.scalar_tensor_tensor(
                out=dT, in0=cur[:, :, 16:80], scalar=1.0 / w, in1=uT[:, :, 16:80], op0=ALU.mult, op1=ALU.subtract),
                reads=[CUR, UT], writes=[DT_])
            C.op("dve", lambda e, cur=cur, g=g: e.tensor_tensor(out=tmp64, in0=cur[:, 0, 16:80], in1=invc[:, g, :], op=ALU.mult),
                 reads=[CUR, KC], writes=[T64])
            C.op("dve", lambda e: e.tensor_tensor(out=dT[:, 0, :], in0=tmp64, in1=uT[:, 0, 16:80], op=ALU.subtract),
                 reads=[T64, UT], writes=[DT_])
            for tg in range(4):
                bk = 4 + (tg % 2)
                pp = bank(bk)
                C.group("pe", [lambda e, pp=pp, g=g, tg=tg: e.matmul(pp, pwb[:, g, :], dT_f[:, tg * 512:(tg + 1) * 512],
                                                                     start=True, stop=True)],
                        reads=[PW, DT_], writes=[PB[bk]])
                C.op("act", lambda e, pp=pp, g=g, tg=tg: e.activation(
                    out=pmT[:, g, tg * 512:(tg + 1) * 512], in_=pp, func=AF.Copy, scale=psc[:, g:g + 1]),
                    reads=[PB[bk], KC], writes=[PM])
        dump("qaug", qaug, QA); dump("pmT", pmT, [PM]); dump("h_own", h_own_f, HOK)
        C.barrier()
        if stop_here("B"):
            return nc
        A.release("wu", "pwb",
                  "uT", "sA", "sB", "dT", "tmp64", "xa0", "xa1", "xa2", "xs0", "xs1", "xs2", "xs3", "xs4", "xs5")

        attn_o = A.alloc("attn_o", [128, 16, 512], BF16)
        AO = Buf()
        kaug = [A.alloc(f"kaug{i}", [96, S], BF16) for i in range(2)]
        KA = [Buf() for _ in range(2)]
        vaug = [A.alloc(f"vaug{i}", [128, 64, 65], BF16) for i in range(2)]
        VA = [Buf() for _ in range(2)]
        bnear = [A.alloc(f"bnear{i}", [128, 10, 256], BF16) for i in range(2)]
        BN = [Buf() for _ in range(2)]
        pT = [A.alloc(f"pT{i}", [128, 1024], BF16) for i in range(3)]
        PT = [Buf() for _ in range(3)]
        rsc = A.alloc("rsc", [128, 8], F32)
        RS = [Buf() for _ in range(8)]
        for i in range(2):
            C.dma("pool", kaug[i][64:96, :], e_rows, writes=[KA[i]])
        v_dram3 = v_dram.rearrange("(t p) (h d) -> p t h d", p=128, h=8)
        sn = 0
        on = 0
        for h in range(NH):
            ks, KS = kaug[h % 2], KA[h % 2]
            C.dma("sp", ks[0:64, :], kt_dram[h * 64:(h + 1) * 64, :], reads=[KTD], writes=[KS])
            hp, hh = h // 2, h % 2
            va, VA_ = vaug[h % 2], VA[h % 2]
            for q4 in range(4):
                C.dma("sp", va[:, q4 * 16:(q4 + 1) * 16, :], v_dram3[:, q4 * 16:(q4 + 1) * 16, h, :],
                      reads=[VD], writes=[VA_])
            bn_, BN_ = bnear[h % 2], BN[h % 2]
            C.dma("pool", bn_, bias_near[h], writes=[BN_])
            for g in range(8):
                n0 = 4 * g - 1
                nlast = 4 * g + 3
                blocks = list(range(0, nlast + 1))
                po = [bank(6 + qt)[:, 0:65] for qt in range(2)]
                PO = [PB[6 + qt] for qt in range(2)]
                on += 1

                def emit_S(unit):
                    nonlocal sn
                    pi = sn % 3
                    sn += 1
                    ps2 = psum_t[:, 2 * pi * 512:(2 * pi + 2) * 512]
                    near = unit[0] >= n0
                    fns = []
                    for j, n in enumerate(unit):
                        for kt in range(2):
                            o_ = ps2[:, j * 512 + kt * 256:j * 512 + (kt + 1) * 256]
                            fns.append(lambda e, kt=kt, o_=o_, n=n: e.matmul(
                                o_, ks[:, n * 256 + kt * 128:n * 256 + (kt + 1) * 128],
                                qaug[:, h, g * 256:(g + 1) * 256], start=True, stop=(not near)))
                            if near:
                                fns.append(lambda e, kt=kt, o_=o_, n=n: e.matmul(
                                    o_, ident_b, bn_[:, (n - n0) * 2 + kt, :], start=False, stop=True))
                    C.group("pe", fns, reads=[KS, QA[h], KC] + ([BN_] if near else []), writes=[PB[2 * pi], PB[2 * pi + 1]])
                    w_ = 512 * len(unit)
                    C.op("act", lambda e, ps2=ps2, pi=pi, near=near, w_=w_: e.activation(
                        out=pT[pi][:, 0:w_], in_=ps2[:, 0:w_], func=AF.Exp, bias=(zero_c if near else b31[:, h:h + 1]), scale=1.0),
                        reads=[PB[2 * pi], PB[2 * pi + 1], KC], writes=[PT[pi]])
                    return pi

                def emit_PV(unit, pi):
                    pt_, PT_ = pT[pi], PT[pi]
                    fns = []
                    for j, n in enumerate(unit):
                        for qt in range(2):
                            if qt == 0 and n >= 4 * g + 2:
                                continue
                            lastn = nlast if qt == 1 else 4 * g + 1
                            for kt in range(2):
                                fns.append(lambda e, qt=qt, kt=kt, n=n, j=j, lastn=lastn: e.matmul(
                                    po[qt], pt_[:, j * 512 + kt * 256 + qt * 128:j * 512 + kt * 256 + (qt + 1) * 128],
                                    va[:, n * 2 + kt, :], start=(n == 0 and kt == 0), stop=(n == lastn and kt == 1)))
                    C.group("pe", fns, reads=[PT_, VA_], writes=[PO[0], PO[1]])

                units = []
                for lst in ([n for n in blocks if n < n0], [n for n in blocks if n >= n0]):
                    for i_ in range(0, len(lst), 2):
                        units.append(lst[i_:i_ + 2])
                pend = []
                for u_ in units:
                    pend.append((u_, emit_S(u_)))
                    if len(pend) > 2:
                        emit_PV(*pend.pop(0))
                while pend:
                    emit_PV(*pend.pop(0))
                for qt in range(2):
                    ri = (on * 2 + qt) % 8
                    C.op("dve", lambda e, qt=qt, ri=ri: e.reciprocal(out=rsc[:, ri:ri + 1], in_=po[qt][:, 64:65]),
                         reads=[PO[qt]], writes=[RS[ri]])
                    C.op("dve", lambda e, qt=qt, ri=ri: e.tensor_scalar(
                        out=attn_o[:, g * 2 + qt, h * 64:(h + 1) * 64], in0=po[qt][:, 0:64], scalar1=rsc[:, ri:ri + 1],
                        scalar2=None, op0=ALU.mult), reads=[PO[qt], RS[ri]], writes=[AO])
        dump("attn_o", attn_o, [AO])
        C.barrier()
        if stop_here("C"):
            return nc
        A.release("qaug", "kaug0", "kaug1", "vaug0", "vaug1", "bnear0", "bnear1", "pT0", "pT1", "pT2", "rsc")

        mT = A.alloc("mT", [128, 8, NOWN], BF16)
        MT = Buf()
        wba = A.alloc("wba", [128, 4, D], BF16)
        wbp = A.alloc("wbp", [128, 4, D], BF16)
        wgt = A.alloc("wgt", [128, 8, 2048], BF16)
        WBA, WBP, WGT = Buf(), Buf(), Buf()
        wload(wba, w_ba, 4, [WBA])
        wload(wbp, w_bp, 4, [WBP])
        for i in range(4):
            C.dma("pool", wgt[:, :, i * 512:(i + 1) * 512],
                  w_in[:, 2048 + i * 512:2048 + (i + 1) * 512].rearrange("(k p) n -> p k n", p=128), writes=[WGT])
        aoT = [A.alloc(f"aoT{i}", [128, 4, 512], BF16) for i in range(2)]
        AOT = [Buf() for _ in range(2)]
        sg = [A.alloc(f"sg{i}", [128, 512], F32) for i in range(4)]
        SG = [Buf() for _ in range(4)]
        m1 = [A.alloc(f"m1_{i}", [128, 512], F32) for i in range(2)]
        M1 = [Buf() for _ in range(2)]
        for tg in range(4):
            at, AT = aoT[tg % 2], AOT[tg % 2]
            for fc in range(4):
                bk = fc % 2
                ptb = bank(bk, BF16)
                C.group("pe", [(lambda e, qt=qt, fc=fc, ptb=ptb, tg=tg: e.transpose(
                    ptb[:, qt * 128:(qt + 1) * 128], attn_o[:, tg * 4 + qt, fc * 128:(fc + 1) * 128], ident_b))
                    for qt in range(4)], reads=[AO, KC], writes=[PB[bk]])
                C.op("dve", lambda e, fc=fc, ptb=ptb, at=at: e.tensor_copy(out=at[:, fc, :], in_=ptb[:, 0:512]),
                     reads=[PB[bk]], writes=[AT])
            for ft in range(8):
                i2 = (tg * 8 + ft) % 2
                pa, pga, pp, pgp = bank(2), bank(3), bank(4), bank(5)
                C.group("pe", [(lambda e, fc=fc, ft=ft: e.matmul(pa, wba[:, fc, ft * 128:(ft + 1) * 128], at[:, fc, :],
                                                                  start=(fc == 0), stop=(fc == 3))) for fc in range(4)],
                        reads=[WBA, AT], writes=[PB[2]])
                C.group("pe", [(lambda e, kc=kc, ft=ft, tg=tg: e.matmul(
                    pga, wgt[:, kc, ft * 128:(ft + 1) * 128], h_own[:, kc, tg * 8:(tg + 1) * 8, 16:80],
                    start=(kc == 0), stop=(kc == 7))) for kc in range(8)], reads=[WGT] + HOK, writes=[PB[3]])
                C.group("pe", [(lambda e, g=g, ft=ft, tg=tg: e.matmul(
                    pp, wbp[:, g, ft * 128:(ft + 1) * 128], pmT[:, g, tg * 512:(tg + 1) * 512],
                    start=(g == 0), stop=(g == 3))) for g in range(4)], reads=[WBP, PM], writes=[PB[4]])
                C.group("pe", [(lambda e, kc=kc, ft=ft, tg=tg: e.matmul(
                    pgp, wgt[:, kc, 1024 + ft * 128:1024 + (ft + 1) * 128], h_own[:, kc, tg * 8:(tg + 1) * 8, 16:80],
                    start=(kc == 0), stop=(kc == 7))) for kc in range(8)], reads=[WGT] + HOK, writes=[PB[5]])
                sa, SA2 = sg[2 * i2], SG[2 * i2]
                sp_, SP2 = sg[2 * i2 + 1], SG[2 * i2 + 1]
                C.op("act", lambda e, sa=sa, ft=ft: e.activation(out=sa, in_=pga, func=AF.Sigmoid, bias=bgc[:, ft:ft + 1], scale=1.0),
                     reads=[PB[3], KC], writes=[SA2])
                C.op("act", lambda e, sp_=sp_, ft=ft: e.activation(out=sp_, in_=pgp, func=AF.Sigmoid, bias=bgc[:, 8 + ft:9 + ft], scale=1.0),
                     reads=[PB[5], KC], writes=[SP2])
                mm, MM = m1[i2], M1[i2]
                C.op("dve", lambda e, mm=mm, sa=sa: e.tensor_tensor(out=mm, in0=pa, in1=sa, op=ALU.mult),
                     reads=[PB[2], SA2], writes=[MM])
                C.op("dve", lambda e, sp_=sp_: e.tensor_tensor(out=sp_, in0=pp, in1=sp_, op=ALU.mult),
                     reads=[PB[4], SP2], writes=[SP2])
                C.op("pool", lambda e, mm=mm, sp_=sp_, ft=ft, tg=tg: e.tensor_tensor(
                    out=mT[:, ft, tg * 512:(tg + 1) * 512], in0=mm, in1=sp_, op=ALU.add),
                    reads=[MM, SP2], writes=[MT])
        dump("mT", mT, [MT])
        C.barrier()
        if stop_here("P1"):
            return nc
        A.release("h_own", "pmT", "attn_o", "wba", "wbp", "wgt", "aoT0", "aoT1", "sg0", "sg1", "sg2", "sg3", "m1_0", "m1_1")

        h2T = A.alloc("h2T", [128, 8, NOWN], BF16)
        H2 = Buf()
        xa = [A.alloc(f"xa{i}", [128, D], F32) for i in range(3)]
        XA = [Buf() for _ in range(3)]
        wo = A.alloc("wo", [128, 8, D], BF16)
        WO = Buf()
        wload(wo, w_out, 8, [WO])
        wr_sb = A.alloc("wr_sb", [128, 8, 36], F32)
        WR = Buf()
        C.dma("sp", wr_sb, w_r.rearrange("(k p) n -> p k n", p=128), writes=[WR])
        x1t = [A.alloc(f"x1t{i}", [128, D], F32) for i in range(2)]
        X1 = [Buf() for _ in range(2)]
        xsf = [A.alloc(f"xsf{i}", [128, D], F32) for i in range(2)]
        XSF = [Buf() for _ in range(2)]
        h2f = [A.alloc(f"h2f{i}", [128, 8, 128], F32) for i in range(2)]
        H2F = [Buf() for _ in range(2)]
        zall = A.alloc("zall", [128, 16, 36], F32)
        ZA = Buf()
        rv = A.alloc("rv", [128, 5, 16], F32)
        r4a = A.alloc("r4a", [128, 16, 4], F32)
        r4b = A.alloc("r4b", [128, 16, 4], F32)
        r32a = A.alloc("r32a", [128, 16, 32], F32)
        r32b = A.alloc("r32b", [128, 16, 32], F32)
        X1D = Buf()
        wg = [A.alloc(f"wg{i}", [128, 8, 512], BF16) for i in range(2)]
        wu2 = [A.alloc(f"wu2{i}", [128, 8, 512], BF16) for i in range(2)]
        wd = [A.alloc(f"wd{i}", [128, 4, D], BF16) for i in range(2)]
        WG, WU2, WD = [Buf(), Buf()], [Buf(), Buf()], [Buf(), Buf()]
        for ex in range(2):
            wload(wg[ex], w_eg[ex], 8, [WG[ex]])
            wload(wu2[ex], w_eu[ex], 8, [WU2[ex]])
            wload(wd[ex], w_ed[ex], 4, [WD[ex]])
        def p2_X(qtile):
            i2 = qtile % 2
            xo, XO = xa[qtile % 3], XA[qtile % 3]
            for half in range(2):
                ch = 2 * qtile + half
                C.dma("sp", xo[half * 64:(half + 1) * 64, :], x_own[ch * 80 + 16:ch * 80 + 80, :], writes=[XO])
            x1, X1_ = x1t[i2], X1[i2]
            for half in range(2):
                pm_ = bank(half)
                C.group("pe", [(lambda e, ft=ft, half=half, pm_=pm_: e.matmul(
                    pm_, mT[:, ft, qtile * 128:(qtile + 1) * 128], wo[:, ft, half * 512:(half + 1) * 512],
                    start=(ft == 0), stop=(ft == 7))) for ft in range(8)], reads=[MT, WO], writes=[PB[half]])
                C.op("dve", lambda e, half=half, pm_=pm_, x1=x1: e.tensor_tensor(
                    out=x1[:, half * 512:(half + 1) * 512], in0=pm_, in1=gate1_rep[:, half * 512:(half + 1) * 512], op=ALU.mult),
                    reads=[PB[half], BM2], writes=[X1_])
            C.op("dve", lambda e, x1=x1, xo=xo: e.tensor_tensor(out=x1, in0=x1, in1=xo, op=ALU.add),
                 reads=[XO, X1_], writes=[X1_])
            C.dma("sp", x1_dram[qtile * 128:(qtile + 1) * 128, :], x1, reads=[X1_], writes=[X1D])
            s4 = stat[:, qtile % 8, :]
            SB_ = ST[qtile % 8]
            C.op("act", lambda e, x1=x1, s4=s4: e.activation(out=junk, in_=x1, func=AF.Square, accum_out=s4[:, 0:1]),
                 reads=[X1_], writes=[SB_])
            C.op("act", lambda e, s4=s4: e.activation(out=s4[:, 1:2], in_=s4[:, 0:1], func=AF.Sqrt, scale=1.0 / D, bias=eps_c),
                 reads=[SB_, KC], writes=[SB_])
            C.op("dve", lambda e, s4=s4: e.reciprocal(out=s4[:, 2:3], in_=s4[:, 1:2]), reads=[SB_], writes=[SB_])
            xf, XF = xsf[i2], XSF[i2]
            C.op("dve", lambda e, xf=xf, x1=x1, s4=s4: e.tensor_scalar(out=xf, in0=x1, scalar1=s4[:, 2:3], scalar2=None, op0=ALU.mult),
                 reads=[X1_, SB_], writes=[XF])

        def p2_Y(qtile):
            i2 = qtile % 2
            xf, XF = xsf[i2], XSF[i2]
            hf, HF = h2f[i2], H2F[i2]
            for hb in range(2):
                bk = 2 + hb
                ptf = bank(bk).rearrange("p (a b) -> p a b", a=4)
                C.group("pe", [(lambda e, k4=k4, hb=hb, ptf=ptf, xf=xf: e.transpose(
                    ptf[:, k4, :], xf[:, (hb * 4 + k4) * 128:(hb * 4 + k4 + 1) * 128], ident_f)) for k4 in range(4)],
                    reads=[XF, KC], writes=[PB[bk]])
                C.op("dve", lambda e, hb=hb, ptf=ptf, hf=hf: e.tensor_tensor(
                    out=hf[:, hb * 4:(hb + 1) * 4, :], in0=ptf, in1=gm2[:, hb * 4:(hb + 1) * 4].unsqueeze(2).to_broadcast([128, 4, 128]),
                    op=ALU.mult), reads=[PB[bk], BM2], writes=[HF])
            C.op("dve", lambda e, hf=hf: e.tensor_tensor(out=hf, in0=hf, in1=sh2.unsqueeze(2).to_broadcast([128, 8, 128]), op=ALU.add),
                 reads=[HF, BM2], writes=[HF])
            C.op("act", lambda e, hf=hf: e.activation(out=h2T[:, :, qtile * 128:(qtile + 1) * 128], in_=hf, func=AF.Copy),
                 reads=[HF], writes=[H2])
            pz = bank(4 + i2)
            C.group("pe", [(lambda e, kc=kc, pz=pz, hf=hf: e.matmul(pz[:, 0:36], hf[:, kc, :], wr_sb[:, kc, :],
                                                                     start=(kc == 0), stop=(kc == 7))) for kc in range(8)],
                    reads=[HF, WR], writes=[PB[4 + i2]])
            C.op("dve", lambda e, pz=pz: e.tensor_tensor(out=zall[:, qtile, :], in0=pz[:, 0:36], in1=b_r_rep, op=ALU.add),
                 reads=[PB[4 + i2], KC], writes=[ZA])
        p2_X(0)
        for qtile in range(16):
            if qtile + 1 < 16:
                p2_X(qtile + 1)
            p2_Y(qtile)
        zg = zall[:, :, 0:4]
        ze = zall[:, :, 4:36]
        bc4 = lambda v: v.unsqueeze(2).to_broadcast([128, 16, 4])
        bc32 = lambda v: v.unsqueeze(2).to_broadcast([128, 16, 32])
        RB = Buf()
        C.op("dve", lambda e: e.tensor_reduce(out=rv[:, 0, :], in_=zg, axis=AX.X, op=ALU.max), reads=[ZA], writes=[RB])
        C.op("dve", lambda e: e.tensor_tensor(out=r4a, in0=zg, in1=bc4(rv[:, 0, :]), op=ALU.subtract), reads=[ZA, RB], writes=[RB])
        C.op("act", lambda e: e.activation(out=r4b, in_=r4a, func=AF.Exp), reads=[RB], writes=[RB])
        C.op("dve", lambda e: e.tensor_reduce(out=rv[:, 1, :], in_=r4b, axis=AX.X, op=ALU.add), reads=[RB], writes=[RB])
        C.op("dve", lambda e: e.reciprocal(out=rv[:, 1, :], in_=rv[:, 1, :]), reads=[RB], writes=[RB])
        C.op("dve", lambda e: e.tensor_scalar(out=r4a, in0=r4a, scalar1=0.0, scalar2=1e30, op0=ALU.is_lt, op1=ALU.mult),
             reads=[RB], writes=[RB])
        C.op("dve", lambda e: e.tensor_tensor(
            out=r32a.rearrange("p t (a b) -> p t a b", a=4), in0=ze.rearrange("p t (a b) -> p t a b", a=4),
            in1=r4a.unsqueeze(3).to_broadcast([128, 16, 4, 8]), op=ALU.subtract), reads=[ZA, RB], writes=[RB])
        C.op("dve", lambda e: e.tensor_reduce(out=rv[:, 2, :], in_=r32a, axis=AX.X, op=ALU.max), reads=[RB], writes=[RB])
        C.op("dve", lambda e: e.tensor_tensor(out=r32b, in0=r32a, in1=bc32(rv[:, 2, :]), op=ALU.is_ge), reads=[RB], writes=[RB])
        C.op("dve", lambda e: e.scalar_tensor_tensor(out=r32b, in0=r32b, scalar=-1e30, in1=r32a, op0=ALU.mult, op1=ALU.add),
             reads=[RB], writes=[RB])
        C.op("dve", lambda e: e.tensor_reduce(out=rv[:, 3, :], in_=r32b, axis=AX.X, op=ALU.max), reads=[RB], writes=[RB])
        C.op("dve", lambda e: e.tensor_tensor(out=r32b, in0=r32a, in1=bc32(rv[:, 3, :]), op=ALU.is_ge), reads=[RB], writes=[RB])
        C.op("dve", lambda e: e.tensor_tensor(out=r32a, in0=r32a, in1=bc32(rv[:, 2, :]), op=ALU.subtract), reads=[RB], writes=[RB])
        C.op("act", lambda e: e.activation(out=r32a, in_=r32a, func=AF.Exp), reads=[RB], writes=[RB])
        C.op("dve", lambda e: e.tensor_tensor(out=r32a, in0=r32a, in1=r32b, op=ALU.mult), reads=[RB], writes=[RB])
        C.op("dve", lambda e: e.tensor_reduce(out=rv[:, 4, :], in_=r32a, axis=AX.X, op=ALU.add), reads=[RB], writes=[RB])
        C.op("dve", lambda e: e.reciprocal(out=rv[:, 4, :], in_=rv[:, 4, :]), reads=[RB], writes=[RB])
        C.op("dve", lambda e: e.tensor_tensor(out=rv[:, 4, :], in0=rv[:, 4, :], in1=rv[:, 1, :], op=ALU.mult), reads=[RB], writes=[RB])
        C.op("dve", lambda e: e.tensor_tensor(out=comb, in0=r32a, in1=bc32(rv[:, 4, :]), op=ALU.mult), reads=[RB], writes=[CB])
        dump("h2T", h2T, [H2]); dump("comb", comb, [CB])
        C.barrier()
        if stop_here("P2"):
            return nc
        A.release("mT", "wo", "wr_sb", "x1t0", "x1t1", "xsf0", "xsf1", "h2f0", "h2f1", "zall", "rv", "r4a", "r4b", "r32a", "r32b")

        yq = [A.alloc(f"yacc{i}", [128, 4, D], F32) for i in range(4)]
        YA = [Buf() for _ in range(16)]
        aT = [A.alloc(f"aT{i}", [128, 4, 512], BF16) for i in range(2)]
        AT2 = [Buf() for _ in range(2)]
        sgl = [A.alloc(f"sgl{i}", [128, 512], F32) for i in range(2)]
        SGL = [Buf() for _ in range(2)]
        for i in range(4):
            C.op("pool", lambda e, i=i: e.memset(yq[i].rearrange("p a b -> p (a b)"), 0.0), writes=YA[4 * i:4 * i + 4])
        def final_tile(tile):
            xo, XO = xa[tile % 3], XA[tile % 3]
            C.dma("sp", xo, x1_dram[tile * 128:(tile + 1) * 128, :], reads=[X1D], writes=[XO])
            yt = yq[tile // 4][:, tile % 4, :]
            C.op("dve", lambda e, yt=yt: e.tensor_tensor(out=yt, in0=yt, in1=gate2_rep, op=ALU.mult),
                 reads=[YA[tile], BM2], writes=[YA[tile]])
            C.op("pool", lambda e, yt=yt, xo=xo: e.tensor_tensor(out=yt, in0=yt, in1=xo, op=ALU.add),
                 reads=[YA[tile], XO], writes=[YA[tile]])
            s4 = stat[:, tile % 8, :]
            SB_ = ST[tile % 8]
            C.op("act", lambda e, yt=yt, s4=s4: e.activation(out=junk, in_=yt, func=AF.Square, accum_out=s4[:, 0:1]),
                 reads=[YA[tile]], writes=[SB_])
            C.op("act", lambda e, s4=s4: e.activation(out=s4[:, 1:2], in_=s4[:, 0:1], func=AF.Sqrt, scale=1.0 / D, bias=eps_c),
                 reads=[SB_, KC], writes=[SB_])
            C.op("dve", lambda e, s4=s4: e.reciprocal(out=s4[:, 2:3], in_=s4[:, 1:2]), reads=[SB_], writes=[SB_])
            C.op("dve", lambda e, yt=yt, s4=s4: e.scalar_tensor_tensor(
                out=yt, in0=yt, scalar=s4[:, 2:3], in1=gf_rep, op0=ALU.mult, op1=ALU.mult),
                reads=[YA[tile], SB_, KC], writes=[YA[tile]])
            C.dma("sp", out_d[tile * 128:(tile + 1) * 128, :], yt, reads=[YA[tile]], is_output=True)

        an = 0
        sn2 = 0
        yn = 0
        for ex in range(NEXP):
            s_ = ex % 2
            if ex >= 2:
                wload(wg[s_], w_eg[ex], 8, [WG[s_]])
                wload(wu2[s_], w_eu[ex], 8, [WU2[s_]])
                wload(wd[s_], w_ed[ex], 4, [WD[s_]])
            for tg in range(4):
                at, AT_ = aT[an % 2], AT2[an % 2]
                an += 1
                for f in range(4):
                    bg_, bu_ = 0 + (sn2 % 2), 2 + (sn2 % 2)
                    pg_, pu_ = bank(bg_), bank(bu_)
                    sl, SL = sgl[sn2 % 2], SGL[sn2 % 2]
                    sn2 += 1
                    C.group("pe", [(lambda e, kc=kc, f=f, tg=tg, pg_=pg_: e.matmul(
                        pg_, wg[s_][:, kc, f * 128:(f + 1) * 128], h2T[:, kc, tg * 512:(tg + 1) * 512],
                        start=(kc == 0), stop=(kc == 7))) for kc in range(8)], reads=[WG[s_], H2], writes=[PB[bg_]])
                    C.group("pe", [(lambda e, kc=kc, f=f, tg=tg, pu_=pu_: e.matmul(
                        pu_, wu2[s_][:, kc, f * 128:(f + 1) * 128], h2T[:, kc, tg * 512:(tg + 1) * 512],
                        start=(kc == 0), stop=(kc == 7))) for kc in range(8)], reads=[WU2[s_], H2], writes=[PB[bu_]])
                    C.op("act", lambda e, sl=sl, pg_=pg_: e.activation(out=sl, in_=pg_, func=AF.Silu), reads=[PB[bg_]], writes=[SL])
                    C.op("dve", lambda e, sl=sl, pu_=pu_, at=at, f=f: e.tensor_tensor(out=at[:, f, :], in0=pu_, in1=sl, op=ALU.mult),
                         reads=[PB[bu_], SL], writes=[AT_])
                for t in range(4):
                    tile = tg * 4 + t
                    for half in range(2):
                        by = 4 + (yn % 4)
                        yn += 1
                        py = bank(by)
                        C.group("pe", [(lambda e, fc=fc, t=t, half=half, py=py, at=at: e.matmul(
                            py, at[:, fc, t * 128:(t + 1) * 128], wd[s_][:, fc, half * 512:(half + 1) * 512],
                            start=(fc == 0), stop=(fc == 3))) for fc in range(4)], reads=[AT_, WD[s_]], writes=[PB[by]])
                        C.op("dve", lambda e, py=py, tile=tile, half=half, ex=ex: e.scalar_tensor_tensor(
                            out=yq[tile // 4][:, tile % 4, half * 512:(half + 1) * 512], in0=py, scalar=comb[:, tile, ex:ex + 1],
                            in1=yq[tile // 4][:, tile % 4, half * 512:(half + 1) * 512], op0=ALU.mult, op1=ALU.add),
                            reads=[PB[by], CB, YA[tile]], writes=[YA[tile]])
                    if ex == NEXP - 1 and half == 1:
                        final_tile(tile)
        C.finish()
    return nc


def t5_bucket_np(rel):
    n = np.maximum(rel, 0)
    nf = np.maximum(n, 1).astype(np.float32)
    large = 16 + (np.log(nf / 16) / math.log(128 / 16) * 16).astype(np.int32)
    large = np.minimum(large, 31)
    return np.where(n < 16, n, large)


def host_inputs(inp, c):
    b, j = c // 4, c % 4
    f = np.float32
    x = inp["x"]
    xb = np.ascontiguousarray(x[b])
    pos = (np.arange(32)[:, None] * 256 + 64 * j - 16 + np.arange(80)[None, :]).reshape(-1)
    x_own = np.zeros((2560, D), f)
    ok = pos >= 0
    x_own[ok] = xb[pos[ok]]
    col = lambda v: np.ascontiguousarray(np.asarray(v, f).reshape(-1, 128).T)
    rel_bias = np.asarray(inp["rel_bias"], f)
    rel = np.arange(5)[:, None, None, None, None] - 1
    kt = np.arange(2)[None, :, None, None, None]
    kp = np.arange(128)[None, None, :, None, None]
    cq = np.arange(4)[None, None, None, :, None]
    r = np.arange(64)[None, None, None, None, :]
    d = (cq - rel) * 256 + 64 * j + r - kt * 128 - kp
    bucket = t5_bucket_np(d)
    bn = np.empty((NH, 128, 10, 256), f)
    for h in range(NH):
        tb = np.where(d >= 0, rel_bias[bucket, h], f(NEG))
        bn[h] = tb.transpose(2, 0, 1, 3, 4).reshape(128, 10, 256)
    e_rows = (np.arange(S)[None, :] // 256 == np.arange(32)[:, None]).astype(f)
    iq = (2 * np.arange(16)[None, :] + (np.arange(128)[:, None] // 64))
    nn = np.arange(32)[None, None, :]
    negmask = np.where(nn >= iq[:, :, None], f(-1e30), f(0)).astype(f)
    valid01 = (nn < iq[:, :, None]).astype(f)
    ownm1 = (nn == iq[:, :, None]).astype(f) - 1.0
    halo = np.full((128, 16), 0.0 if j == 0 else 1.0, f)
    t = 64 * j + np.arange(64)
    invc = np.stack([1.0 / np.minimum(t + 1, w) for w in (2, 4, 8, 16)], 0).astype(f)
    invc = np.broadcast_to(invc[None], (128, 4, 64)).copy()
    m = {
        "x_all": xb, "x_own": x_own,
        "w_in": np.ascontiguousarray(inp["w_in"][0]), "w_ada": np.ascontiguousarray(inp["w_ada"][0]),
        "b_ada": np.ascontiguousarray(inp["b_ada"][0][None, :]),
        "ccol": col(inp["c"][b]), "g1col": col(inp["norm1_g"][0]), "g2col": col(inp["norm2_g"][0]),
        "gf_rep": np.broadcast_to(np.asarray(inp["norm_f_g"], f)[None, :], (128, D)).copy(),
        "bgate_col": col(inp["b_gate"][0]), "pscale_col": col(inp["pool_scale"][0]),
        "b31_rep": np.broadcast_to(rel_bias[31][None, :], (128, 8)).copy(),
        "pool_w": np.ascontiguousarray(inp["pool_w"][0]),
        "w_ba": np.ascontiguousarray(inp["w_branch_attn"][0]), "w_bp": np.ascontiguousarray(inp["w_branch_pool"][0]),
        "w_out": np.ascontiguousarray(inp["w_out"][0]),
        "w_r": np.ascontiguousarray(np.concatenate([inp["w_router_group"][0], inp["w_router_expert"][0]], axis=1)),
        "b_r_rep": np.broadcast_to(np.concatenate([inp["b_router_group"][0], inp["b_router_expert"][0]])[None, :],
                                   (128, 36)).astype(f).copy(),
        "w_eg": np.ascontiguousarray(inp["w_expert_gate"][0]), "w_eu": np.ascontiguousarray(inp["w_expert_up"][0]),
        "w_ed": np.ascontiguousarray(inp["w_expert_down"][0]),
        "bias_near": bn, "e_rows": e_rows, "negmask": negmask, "valid01": valid01, "ownm1": ownm1.astype(f),
        "halo_mask": halo, "invcnt0": invc, "ident": np.eye(128, dtype=f),
    }
    return {k: np.ascontiguousarray(np.asarray(v, f)) for k, v in m.items()}


def kernel(**inputs):
    inp = {k: np.asarray(v) for k, v in inputs.items()}
    nc = build_program()
    in_maps = [host_inputs(inp, c) for c in range(8)]
    res = run_bass_kernel_spmd(nc, in_maps, core_ids=list(range(8)))
    out = np.empty((2, S, D), np.float32)
    for c in range(8):
        b, j = c // 4, c % 4
        o = np.asarray(res.results[c]["out"]).reshape(32, 64, D)
        out[b].reshape(32, 256, D)[:, 64 * j:64 * j + 64, :] = o
    return out
```

```python
import math
from contextlib import ExitStack
import numpy as np
import concourse.bass as bass
import concourse.mybir as mybir
from concourse.bass_utils import run_bass_kernel_spmd

F32 = mybir.dt.float32
BF16 = mybir.dt.bfloat16
AF = mybir.ActivationFunctionType
ALU = mybir.AluOpType
AX = mybir.AxisListType

D = 1024
S = 8192
NOWN = 2048
NH = 8
NEG = -30000.0
NEXP = 32


class Buf:
    __slots__ = ("name", "w", "r", "excl")

    def __init__(self, name="", excl=False):
        self.name = name
        self.w = None
        self.r = {}
        self.excl = excl


class Eng:
    def __init__(self, name, h, sem):
        self.name, self.h, self.sem, self.cnt, self.seen = name, h, sem, 0, {}


class Ctx:
    def __init__(self, nc, stack, n_dma_slots=8):
        self.nc = nc
        self.E = {}
        for name, h in (("pe", nc.tensor), ("act", nc.scalar), ("dve", nc.vector),
                        ("pool", nc.gpsimd), ("sp", nc.sync)):
            self.E[name] = Eng(name, h, stack.enter_context(nc.semaphore("sem_" + name)))
        self.dma_slots = {}
        for q in ("sp", "pool"):
            sl = [[stack.enter_context(nc.semaphore(f"dq_{q}_{i}")), 0] for i in range(n_dma_slots)]
            self.dma_slots[q] = [sl, 0]
        self.out_events = []

    def _wait(self, eng, ev):
        if ev is None:
            return
        key, sem, val = ev
        if key == eng.name and eng.name == "pe":
            return
        if eng.seen.get(key, 0) >= val:
            return
        eng.seen[key] = val
        eng.h.wait_ge(sem, val)

    @staticmethod
    def _split(reads, writes):
        return list(reads), list(writes)

    def _deps(self, eng, reads, writes):
        for b in reads:
            self._wait(eng, b.w)
            if b.excl:
                for key, (sem, val) in list(b.r.items()):
                    if key != eng.name:
                        self._wait(eng, (key, sem, val))
        for b in writes:
            self._wait(eng, b.w)
            for key, (sem, val) in list(b.r.items()):
                self._wait(eng, (key, sem, val))

    def _commit(self, ev, reads, writes):
        key, sem, val = ev
        for b in reads:
            b.r[key] = (sem, val)
        for b in writes:
            b.w = ev
            b.r = {}

    def op(self, en, fn, reads=(), writes=()):
        eng = self.E[en]
        reads, writes = self._split(reads, writes)
        self._deps(eng, reads, writes)
        eng.cnt += 1
        fn(eng.h).then_inc(eng.sem, 1)
        self._commit((eng.name, eng.sem, eng.cnt), reads, writes)

    def group(self, en, fns, reads=(), writes=()):
        eng = self.E[en]
        reads, writes = self._split(reads, writes)
        self._deps(eng, reads, writes)
        for f in fns[:-1]:
            f(eng.h)
        eng.cnt += 1
        fns[-1](eng.h).then_inc(eng.sem, 1)
        self._commit((eng.name, eng.sem, eng.cnt), reads, writes)

    def dma(self, q, out, in_, reads=(), writes=(), is_output=False, **kw):
        eng = self.E[q]
        slots, idx = self.dma_slots[q]
        k = idx % len(slots)
        slot = slots[k]
        self.dma_slots[q][1] = idx + 1
        key = f"dq_{q}_{k}"
        if slot[1] > 0:
            self._wait(eng, (key, slot[0], slot[1]))
        self._deps(eng, reads, writes)
        slot[1] += 16
        eng.h.dma_start(out=out, in_=in_, **kw).then_inc(slot[0], 16)
        ev = (key, slot[0], slot[1])
        self._commit(ev, reads, writes)
        if is_output:
            self.out_events.append(ev)

    def barrier(self):
        evs = [(e.name, e.sem, e.cnt) for e in self.E.values() if e.cnt > 0]
        for q, (slots, idx) in self.dma_slots.items():
            for i, sl in enumerate(slots):
                if sl[1] > 0:
                    evs.append((f"dq_{q}_{i}", sl[0], sl[1]))
        for e in self.E.values():
            for ev in evs:
                self._wait(e, ev)

    def finish(self):
        eng = self.E["sp"]
        for ev in self.out_events:
            self._wait(eng, ev)
        for q, (slots, idx) in self.dma_slots.items():
            for i, sl in enumerate(slots):
                if sl[1] > 0:
                    self._wait(eng, (f"dq_{q}_{i}", sl[0], sl[1]))


class Arena:
    def __init__(self, ap, nbytes):
        self.ap = ap
        self.free = [(0, nbytes)]
        self.live = {}

    def alloc(self, name, shape, dt):
        esz = 4 if dt == F32 else 2
        n = esz
        for s in shape[1:]:
            n *= s
        n = (n + 63) // 64 * 64
        for i, (o, sz) in enumerate(self.free):
            if sz >= n:
                self.free[i] = (o + n, sz - n)
                self.live[name] = (o, n)
                v = self.ap[0:shape[0], o // 2:(o + n) // 2]
                if dt == F32:
                    v = v.bitcast(F32)
                nel = 1
                for s in shape[1:]:
                    nel *= s
                v = v[:, 0:nel]
                if len(shape) == 3:
                    v = v.rearrange("p (a b) -> p a b", a=shape[1])
                elif len(shape) == 4:
                    v = v.rearrange("p (a b c) -> p a b c", a=shape[1], b=shape[2])
                return v
        raise RuntimeError(f"arena OOM for {name} {shape} free={self.free}")

    def release(self, *names):
        for name in names:
            o, n = self.live.pop(name)
            self.free.append((o, n))
        self.free.sort()
        m = []
        for o, n in self.free:
            if n == 0:
                continue
            if m and m[-1][0] + m[-1][1] == o:
                m[-1] = (m[-1][0], m[-1][1] + n)
            else:
                m.append((o, n))
        self.free = m


def build_program(stop=None, debug=False, opts=None):
    opts = opts or {}
    nc = bass.Bass("TRN2", target_bir_lowering=False)
    skind = "ExternalOutput" if debug else "Internal"

    def din(name, shape, dt=F32):
        return nc.dram_tensor(name, list(shape), dt, kind="ExternalInput").ap()

    x_all = din("x_all", [S, D])
    x_own = din("x_own", [2560, D])
    w_in = din("w_in", [D, 4096])
    w_ada = din("w_ada", [D, 6144])
    b_ada = din("b_ada", [1, 6144])
    ccol = din("ccol", [128, 8])
    g1col = din("g1col", [128, 8])
    g2col = din("g2col", [128, 8])
    gf_rep_d = din("gf_rep", [128, D])
    bgate_col = din("bgate_col", [128, 16])
    pscale_col = din("pscale_col", [128, 4])
    b31_rep_d = din("b31_rep", [128, 8])
    pool_w = din("pool_w", [4, 128, 128])
    w_ba = din("w_ba", [512, D])
    w_bp = din("w_bp", [512, D])
    w_out = din("w_out", [D, D])
    w_r = din("w_r", [D, 36])
    b_r_rep_d = din("b_r_rep", [128, 36])
    w_eg = din("w_eg", [NEXP, D, 512])
    w_eu = din("w_eu", [NEXP, D, 512])
    w_ed = din("w_ed", [NEXP, 512, D])
    bias_near = din("bias_near", [NH, 128, 10, 256])
    e_rows = din("e_rows", [32, S])
    negmask_d = din("negmask", [128, 16, 32])
    valid_d = din("valid01", [128, 16, 32])
    ownm1_d = din("ownm1", [128, 16, 32])
    halo_d = din("halo_mask", [128, 16])
    invc_d = din("invcnt0", [128, 4, 64])
    ident_d = din("ident", [128, 128])
    out_d = nc.dram_tensor("out", [NOWN, D], F32, kind="ExternalOutput").ap()
    kt_dram = nc.dram_tensor("kt_scr", [512, S], BF16, kind=skind).ap()
    v_dram = nc.dram_tensor("v_scr", [S, 520], BF16, kind=skind).ap()
    x1_dram = nc.dram_tensor("x1_scr", [NOWN, D], F32, kind=skind).ap()

    with ExitStack() as st:
        ARENA_BYTES = 211968
        arena_t = st.enter_context(nc.sbuf_tensor("arena", [128, ARENA_BYTES // 2], BF16))
        psum_t = st.enter_context(nc.psum_tensor("psum", [128, 4096], F32))
        C = Ctx(nc, st)
        A = Arena(arena_t, ARENA_BYTES)

        def bank(k, dt=F32):
            v = psum_t[:, k * 512:(k + 1) * 512]
            return v.bitcast(BF16) if dt == BF16 else v

        PB = [Buf(f"bank{k}", excl=opts.get("excl", True)) for k in range(8)]

        def dump(name, ap, bufs):
            if not debug:
                return
            d = nc.dram_tensor("dbg_" + name, list(ap.shape), ap.dtype, kind="ExternalOutput").ap()
            C.dma("sp", d, ap, reads=bufs, is_output=True)

        def stop_here(tag):
            if stop == tag:
                C.finish()
                return True
            return False

        def wload(dst, src_rows_ap, kcn, bufs):
            C.dma("pool", dst, src_rows_ap.rearrange("(k p) n -> p k n", p=128), writes=bufs)

        ident_f = A.alloc("ident_f", [128, 128], F32)
        ident_b = A.alloc("ident_b", [128, 128], BF16)
        cc = A.alloc("cc", [128, 8], F32)
        g1c = A.alloc("g1c", [128, 8], F32)
        g2c = A.alloc("g2c", [128, 8], F32)
        gf_rep = A.alloc("gf_rep", [128, D], F32)
        bgc = A.alloc("bgc", [128, 16], F32)
        psc = A.alloc("psc", [128, 4], F32)
        b31 = A.alloc("b31", [128, 8], F32)
        b_r_rep = A.alloc("b_r_rep", [128, 36], F32)
        negmask = A.alloc("negmask", [128, 16, 32], F32)
        valid01 = A.alloc("valid01", [128, 16, 32], F32)
        ownm1 = A.alloc("ownm1", [128, 16, 32], F32)
        halo = A.alloc("halo", [128, 16], F32)
        invc = A.alloc("invc", [128, 4, 64], F32)
        zero_c = A.alloc("zero_c", [128, 1], F32)
        eps_c = A.alloc("eps_c", [128, 1], F32)
        ones_r = A.alloc("ones_r", [1, 128], F32)
        modcol = A.alloc("modcol", [128, 4, 8], F32)
        gm1 = A.alloc("gm1", [128, 8], F32)
        gm2 = A.alloc("gm2", [128, 8], F32)
        gate1_rep = A.alloc("gate1_rep", [128, D], F32)
        gate2_rep = A.alloc("gate2_rep", [128, D], F32)
        kmean8 = A.alloc("kmean8", [64, 8, 32], F32)
        comb = A.alloc("comb", [128, 16, 32], F32)
        KC = Buf("consts")
        CB = Buf("comb")
        for dst, src in ((ident_f, ident_d), (cc, ccol), (g1c, g1col), (g2c, g2col), (gf_rep, gf_rep_d),
                         (bgc, bgate_col), (psc, pscale_col), (b31, b31_rep_d), (b_r_rep, b_r_rep_d),
                         (negmask, negmask_d), (valid01, valid_d), (ownm1, ownm1_d), (halo, halo_d),
                         (invc, invc_d)):
            C.dma("sp", dst, src, writes=[KC])
        C.dma("pool", ident_b, ident_d, writes=[KC])
        C.op("dve", lambda e: e.memset(zero_c, 0.0), writes=[KC])
        C.op("dve", lambda e: e.memset(eps_c, 1e-6), writes=[KC])
        C.op("dve", lambda e: e.memset(ones_r, 1.0), writes=[KC])

        dump("negmask", negmask, [KC]); dump("ident_b", ident_b, [KC])
        if stop_here("00"):
            return nc
        cact = A.alloc("cact", [128, 8], F32)
        mod_row = A.alloc("mod_row", [1, 6144], F32)
        bada = A.alloc("bada", [1, 6144], F32)
        wst = [A.alloc(f"wada_st{i}", [128, 8, 512], F32) for i in range(2)]
        WST = [Buf(), Buf()]
        BM = Buf("mod")
        C.dma("sp", bada, b_ada, writes=[BM])
        C.op("act", lambda e: e.activation(out=cact, in_=cc, func=AF.Silu), reads=[KC], writes=[BM])
        def ada_cols(cts):
            for ct in cts:
                s_ = ct % 2
                C.dma("sp", wst[s_], w_ada[:, ct * 512:(ct + 1) * 512].rearrange("(k p) n -> p k n", p=128),
                      writes=[WST[s_]])
                pb = bank(ct % 2)
                C.group("pe", [(lambda e, kc=kc, s_=s_, pb=pb: e.matmul(pb[0:1, :], cact[:, kc:kc + 1], wst[s_][:, kc, :],
                                                                         start=(kc == 0), stop=(kc == 7)))
                               for kc in range(8)], reads=[BM, WST[s_]], writes=[PB[ct % 2]])
                C.op("dve", lambda e, ct=ct, pb=pb: e.tensor_tensor(out=mod_row[:, ct * 512:(ct + 1) * 512], in0=pb[0:1, :],
                                                                     in1=bada[:, ct * 512:(ct + 1) * 512], op=ALU.add),
                     reads=[PB[ct % 2], BM], writes=[BM])

        ada_cols(range(0, 4))
        mod_dram = nc.dram_tensor("mod_scr", [1, 6144], F32, kind=skind).ap()
        MD = Buf()
        C.dma("sp", mod_dram[:, 0:2048], mod_row[:, 0:2048], reads=[BM], writes=[MD])
        for vi, v in ((0, 0), (1, 1)):
            C.dma("sp", modcol[:, vi, :], mod_dram[0, v * 1024:(v + 1) * 1024].rearrange("(k p) -> p k", p=128),
                  reads=[MD], writes=[BM], allow_slow_non_contiguous=True)
        C.op("dve", lambda e: e.scalar_tensor_tensor(out=gm1, in0=modcol[:, 1, :], scalar=1.0, in1=g1c, op0=ALU.add, op1=ALU.mult),
             reads=[BM, KC], writes=[BM])
        sh1 = modcol[:, 0, :]
        sh2 = modcol[:, 2, :]

        def ada_finish():
            MD2 = Buf()
            C.dma("sp", mod_dram[:, 2048:6144], mod_row[:, 2048:6144], reads=[BM], writes=[MD2])
            for vi, v in ((2, 3), (3, 4)):
                C.dma("sp", modcol[:, vi, :], mod_dram[0, v * 1024:(v + 1) * 1024].rearrange("(k p) -> p k", p=128),
                      reads=[MD2], writes=[BM2], allow_slow_non_contiguous=True)
            C.op("dve", lambda e: e.scalar_tensor_tensor(out=gm2, in0=modcol[:, 3, :], scalar=1.0, in1=g2c, op0=ALU.add, op1=ALU.mult),
                 reads=[BM2, KC], writes=[BM2])
            for grep, v in ((gate1_rep, 2), (gate2_rep, 5)):
                C.dma("sp", grep, mod_dram[0:1, v * 1024:(v + 1) * 1024].to_broadcast([128, 1024]), reads=[MD2], writes=[BM2])
            dump("modcol", modcol, [BM, BM2]); dump("gm1", gm1, [BM]); dump("gate1_rep", gate1_rep, [BM2]); dump("gate2_rep", gate2_rep, [BM2])

        BM2 = Buf("mod2")
        if stop == "0":
            ada_cols(range(4, 12))
            ada_finish()
            C.barrier()
            C.finish()
            return nc

        xa = [A.alloc(f"xa{i}", [128, D], F32) for i in range(3)]
        XA = [Buf() for _ in range(3)]
        xs = [A.alloc(f"xs{i}", [128, D], BF16) for i in range(6)]
        XS = [Buf() for _ in range(6)]
        junk = A.alloc("junk", [128, D], BF16)
        JK = Buf()
        stat = A.alloc("stat", [128, 8, 4], F32)
        ST = [Buf() for _ in range(8)]
        ncount = [0]

        def norm_s1(xin, XIN, slot):
            i = ncount[0]
            ncount[0] += 1
            s4 = stat[:, i % 8, :]
            SB_ = ST[i % 8]
            xsb, XSB = xs[slot], XS[slot]
            C.op("act", lambda e: e.activation(out=junk, in_=xin, func=AF.Square, accum_out=s4[:, 0:1]),
                 reads=[XIN], writes=[SB_])
            C.op("act", lambda e: e.activation(out=s4[:, 1:2], in_=s4[:, 0:1], func=AF.Sqrt, scale=1.0 / D, bias=eps_c),
                 reads=[SB_, KC], writes=[SB_])
            C.op("dve", lambda e: e.reciprocal(out=s4[:, 2:3], in_=s4[:, 1:2]), reads=[SB_], writes=[SB_])
            C.op("dve", lambda e: e.tensor_scalar(out=xsb, in0=xin, scalar1=s4[:, 2:3], scalar2=None, op0=ALU.mult),
                 reads=[XIN, SB_], writes=[XSB])

        def norm_s2(slot, gm, sh, out3, OUT, tpbank):
            xsb, XSB = xs[slot], XS[slot]
            tp = bank(tpbank, BF16).rearrange("p (a b) -> p a b", a=8)
            C.group("pe", [(lambda e, kc=kc: e.transpose(tp[:, kc, :], xsb[:, kc * 128:(kc + 1) * 128], ident_b))
                           for kc in range(8)], reads=[XSB, KC], writes=[PB[tpbank]])
            for kc in range(8):
                C.op("act", lambda e, kc=kc: e.activation(out=out3[:, kc, :], in_=tp[:, kc, :], func=AF.Identity,
                                                          scale=gm[:, kc:kc + 1], bias=sh[:, kc:kc + 1]),
                     reads=[PB[tpbank], BM], writes=[OUT[kc]])

        def norm_T(xin, XIN, gm, sh, out3, OUT, tpbank):
            slot = ncount[0] % len(xs)
            norm_s1(xin, XIN, slot)
            norm_s2(slot, gm, sh, out3, OUT, tpbank)

        wkv = A.alloc("wkv", [128, 8, 1024], BF16)
        WKV = Buf()
        wload(wkv, w_in[:, 512:1536], 8, [WKV])
        hT = [A.alloc(f"hT{i}", [128, 8, 512], BF16) for i in range(2)]
        HT = [Buf() for _ in range(2)]
        ktsb = [A.alloc(f"ktsb{i}", [128, 512], BF16) for i in range(2)]
        KTS = [Buf() for _ in range(2)]
        vsb = [A.alloc(f"vsb{i}", [128, 8, 65], BF16) for i in range(2)]
        VSB = [Buf() for _ in range(2)]
        kmsum = A.alloc("kmsum", [128, 4, 32], F32)
        KMS = Buf()
        KTD, VD = Buf("ktd"), Buf("vd")
        for i in range(2):
            C.op("dve", lambda e, i=i: e.memset(vsb[i], 1.0), writes=[VSB[i]])
        HTK = [[Buf() for _ in range(8)] for _ in range(2)]

        def a_s1(tile):
            C.dma("sp", xa[tile % 3], x_all[tile * 128:(tile + 1) * 128, :], writes=[XA[tile % 3]])
            norm_s1(xa[tile % 3], XA[tile % 3], tile % 6)

        def a_s2(tile):
            tg_, t_ = tile // 4, tile % 4
            norm_s2(tile % 6, gm1, sh1, hT[tg_ % 2][:, :, t_ * 128:(t_ + 1) * 128], HTK[tg_ % 2], tile % 2)

        for tile in range(4):
            a_s1(tile)
        for tile in range(4):
            a_s2(tile)
        kcnt = 0
        kbanks = (2, 3, 6)
        vbanks = (4, 5, 7)
        for tg in range(16):
            hs, HS = hT[tg % 2], HTK[tg % 2]
            if tg < 15:
                for t in range(4):
                    a_s1((tg + 1) * 4 + t)
            if tg == 1:
                ada_cols(range(4, 12))
            for hp in range(4):
                bk = kbanks[kcnt % 3]
                pk = bank(bk)
                C.group("pe", [(lambda e, kc=kc, hp=hp, pk=pk, hs=hs: e.matmul(
                    pk, wkv[:, kc, hp * 128:(hp + 1) * 128], hs[:, kc, :], start=(kc == 0), stop=(kc == 7)))
                    for kc in range(8)], reads=[WKV] + HS, writes=[PB[bk]])
                ks, KS = ktsb[kcnt % 2], KTS[kcnt % 2]
                C.op("dve", lambda e, ks=ks, pk=pk: e.tensor_copy(out=ks, in_=pk), reads=[PB[bk]], writes=[KS])
                C.op("dve", lambda e, ks=ks, hp=hp, tg=tg: e.tensor_reduce(
                    out=kmsum[:, hp, 2 * tg:2 * tg + 2], in_=ks.rearrange("p (a b) -> p a b", a=2), axis=AX.X, op=ALU.add),
                    reads=[KS], writes=[KMS])
                C.dma("pool", kt_dram[hp * 128:(hp + 1) * 128, tg * 512:(tg + 1) * 512], ks, reads=[KS], writes=[KTD])
                kcnt += 1
                if tg < 15:
                    a_s2((tg + 1) * 4 + hp)
            for t in range(4):
                tile = tg * 4 + t
                bk = vbanks[tile % 3]
                pv = bank(bk)
                C.group("pe", [(lambda e, kc=kc, t=t, pv=pv, hs=hs: e.matmul(
                    pv, hs[:, kc, t * 128:(t + 1) * 128], wkv[:, kc, 512:1024], start=(kc == 0), stop=(kc == 7)))
                    for kc in range(8)], reads=[WKV] + HS, writes=[PB[bk]])
                vs_, VS_ = vsb[tile % 2], VSB[tile % 2]
                C.op("dve", lambda e, vs_=vs_, pv=pv: e.tensor_copy(
                    out=vs_[:, :, 0:64], in_=pv.rearrange("p (a b) -> p a b", a=8)), reads=[PB[bk]], writes=[VS_])
                C.dma("pool", v_dram[tile * 128:(tile + 1) * 128, :], vs_.rearrange("p a b -> p (a b)"),
                      reads=[VS_], writes=[VD])
        for hp in range(4):
            for hh in range(2):
                C.dma("sp", kmean8[:, 2 * hp + hh, :], kmsum[hh * 64:(hh + 1) * 64, hp, :], reads=[KMS], writes=[KMS])
        ada_finish()
        dump("kmean8", kmean8, [KMS])
        C.barrier()
        if stop_here("A"):
            return nc
        A.release("cact", "mod_row", "bada", "wada_st0", "wada_st1")
        A.release("wkv", "hT0", "hT1", "ktsb0", "ktsb1", "vsb0", "vsb1", "kmsum")

        h_own = A.alloc("h_own", [128, 8, 32, 80], BF16)
        h_own_f = h_own.rearrange("p k c t -> p k (c t)")
        HOK = [Buf() for _ in range(8)]
        qaug = A.alloc("qaug", [96, 8, NOWN], BF16)
        QA = [Buf() for _ in range(8)]
        pmT = A.alloc("pmT", [128, 4, NOWN], BF16)
        PM = Buf()
        wq = A.alloc("wq", [128, 8, 512], BF16)
        wu = A.alloc("wu", [128, 8, 512], BF16)
        pwb = A.alloc("pwb", [128, 4, 128], BF16)
        WQ, WU, PW = Buf(), Buf(), Buf()
        wload(wq, w_in[:, 0:512], 8, [WQ])
        wload(wu, w_in[:, 1536:2048], 8, [WU])
        C.dma("pool", pwb, pool_w.rearrange("g c d -> c g d"), writes=[PW])
        for tile in range(20):
            C.dma("sp", xa[tile % 3], x_own[tile * 128:(tile + 1) * 128, :], writes=[XA[tile % 3]])
            norm_T(xa[tile % 3], XA[tile % 3], gm1, sh1, h_own_f[:, :, tile * 128:(tile + 1) * 128], HOK, tile % 2)
        qtf = [A.alloc(f"qtf{i}", [64, NOWN], F32) for i in range(2)]
        QTF = [Buf() for _ in range(2)]
        mbT = [A.alloc(f"mbT{i}", [32, NOWN], BF16) for i in range(2)]
        MBT = [Buf() for _ in range(2)]
        gA = A.alloc("gA", [128, 16, 32], F32)
        gB = A.alloc("gB", [128, 16, 32], F32)
        gMk = A.alloc("gMk", [128, 16, 32], F32)
        gTb2 = [A.alloc(f"gTb{i}", [128, 16, 32], BF16) for i in range(2)]
        GTB2 = [Buf(), Buf()]
        gmx = A.alloc("gmx", [128, 3, 16], F32)
        GA, GB, GMK, GMX = Buf(), Buf(), Buf(), Buf()
        ptb2 = psum_t[:, 6 * 512:8 * 512].bitcast(BF16)
        qn = 0
        def b_part1(h):
            nonlocal qn
            qf, QF = qtf[h % 2], QTF[h % 2]
            for tg in range(4):
                bk = 2 + (qn % 2)
                pq = bank(bk)
                qn += 1
                C.group("pe", [(lambda e, kc=kc, h=h, tg=tg, pq=pq: e.matmul(
                    pq[0:64, :], wq[:, kc, h * 64:(h + 1) * 64], h_own[:, kc, tg * 8:(tg + 1) * 8, 16:80],
                    start=(kc == 0), stop=(kc == 7))) for kc in range(8)], reads=[WQ] + HOK, writes=[PB[bk]])
                C.op("act", lambda e, h=h, tg=tg, pq=pq: e.activation(
                    out=qaug[0:64, h, tg * 512:(tg + 1) * 512], in_=pq[0:64, :], func=AF.Copy, scale=0.125),
                    reads=[PB[bk]], writes=[QA[h]])
                C.op("dve", lambda e, qf=qf, pq=pq, tg=tg: e.tensor_copy(out=qf[:, tg * 512:(tg + 1) * 512], in_=pq[0:64, :]),
                     reads=[PB[bk]], writes=[QF])
            bg = 4 + (h % 2)
            pg = bank(bg).rearrange("p (a b) -> p a b", a=16)
            C.group("pe", [(lambda e, qt=qt, pg=pg, qf=qf, h=h: e.matmul(
                pg[:, qt, :], qf[:, qt * 128:(qt + 1) * 128], kmean8[:, h, :], start=True, stop=True)) for qt in range(16)],
                reads=[QF, KMS], writes=[PB[bg]])
            C.op("dve", lambda e, pg=pg: e.tensor_tensor(out=gA, in0=pg, in1=negmask, op=ALU.add), reads=[PB[bg], KC], writes=[GA])
            cur, CUR, oth, OTH = gA, GA, gB, GB
            for rnd in range(3):
                C.op("dve", lambda e, cur=cur, rnd=rnd: e.tensor_reduce(out=gmx[:, rnd, :], in_=cur, axis=AX.X, op=ALU.max),
                     reads=[CUR], writes=[GMX])
                if rnd == 2:
                    break
                C.op("dve", lambda e, cur=cur, rnd=rnd: e.tensor_tensor(
                    out=gMk, in0=cur, in1=gmx[:, rnd, :].unsqueeze(2).to_broadcast([128, 16, 32]), op=ALU.is_ge),
                    reads=[CUR, GMX], writes=[GMK])
                C.op("dve", lambda e, cur=cur, oth=oth: e.scalar_tensor_tensor(
                    out=oth, in0=gMk, scalar=-1e30, in1=cur, op0=ALU.mult, op1=ALU.add), reads=[GMK, CUR], writes=[OTH])
                cur, CUR, oth, OTH = oth, OTH, cur, CUR
            C.op("dve", lambda e, pg=pg: e.tensor_tensor(out=gB, in0=pg, in1=negmask, op=ALU.add), reads=[PB[bg], KC], writes=[GB])
            C.op("dve", lambda e: e.tensor_tensor(out=gMk, in0=gB, in1=gmx[:, 2, :].unsqueeze(2).to_broadcast([128, 16, 32]), op=ALU.is_ge),
                 reads=[GB, GMX], writes=[GMK])
            C.op("dve", lambda e: e.tensor_tensor(out=gMk, in0=gMk, in1=valid01, op=ALU.mult), reads=[GMK, KC], writes=[GMK])
            C.op("dve", lambda e, h=h: e.tensor_tensor(out=gTb2[h % 2], in0=gMk, in1=ownm1, op=ALU.add), reads=[GMK, KC], writes=[GTB2[h % 2]])

        def b_part2(h):
            C.group("pe", [(lambda e, qt=qt: e.transpose(ptb2[0:32, qt * 128:(qt + 1) * 128], gTb2[h % 2][:, qt, :], ident_b))
                           for qt in range(16)], reads=[GTB2[h % 2], KC], writes=[PB[6], PB[7]])
            for hb in range(2):
                C.op("act", lambda e, hb=hb, h=h: e.activation(
                    out=mbT[h % 2][:, hb * 1024:(hb + 1) * 1024], in_=ptb2[0:32, hb * 1024:(hb + 1) * 1024], func=AF.Copy,
                    scale=-NEG), reads=[PB[6 + hb]], writes=[MBT[h % 2]])
            C.dma("sp", qaug[64:96, h, :], mbT[h % 2], reads=[MBT[h % 2]], writes=[QA[h]])
        b_part1(0)
        for h in range(NH):
            if h + 1 < NH:
                b_part1(h + 1)
            b_part2(h)
        C.barrier()
        A.release("qtf0", "qtf1", "mbT0", "mbT1", "gA", "gB", "gMk", "gTb0", "gTb1", "gmx", "wq")
        uT = A.alloc("uT", [128, 32, 80], F32)
        uT_f = uT.rearrange("p c t -> p (c t)")
        sA = A.alloc("sA", [128, 32, 80], F32)
        sB = A.alloc("sB", [128, 32, 80], F32)
        dT = A.alloc("dT", [128, 32, 64], BF16)
        dT_f = dT.rearrange("p c t -> p (c t)")
        tmp64 = A.alloc("tmp64", [128, 64], F32)
        UT, SA_, SB2, DT_, T64 = Buf(), Buf(), Buf(), Buf(), Buf()
        pn = 0
        for g in range(4):
            for cg in range(5):
                bk = 2 + (pn % 2)
                pu = bank(bk)
                C.group("pe", [(lambda e, kc=kc, g=g, cg=cg, pu=pu: e.matmul(
                    pu, wu[:, kc, g * 128:(g + 1) * 128], h_own_f[:, kc, cg * 512:(cg + 1) * 512],
                    start=(kc == 0), stop=(kc == 7))) for kc in range(8)], reads=[WU] + HOK, writes=[PB[bk]])
                C.op("act", lambda e, cg=cg, pu=pu: e.activation(out=uT_f[:, cg * 512:(cg + 1) * 512], in_=pu, func=AF.Copy),
                     reads=[PB[bk]], writes=[UT])
                pn += 1
            C.op("dve", lambda e: e.tensor_tensor(out=uT_f[:, 0:16], in0=uT_f[:, 0:16], in1=halo, op=ALU.mult),
                 reads=[UT, KC], writes=[UT])
            cur, CUR = uT, UT
            nxt = [(sA, SA_), (sB, SB2)]
            sh_ = 1
            for k in range(g + 1):
                dst, DST = nxt[k % 2]
                lo = 2 * sh_ - 1
                C.op("pool", lambda e, dst=dst, cur=cur, lo=lo, sh_=sh_: e.tensor_tensor(
                    out=dst[:, :, lo:80], in0=cur[:, :, lo:80], in1=cur[:, :, lo - sh_:80 - sh_], op=ALU.add),
                    reads=[CUR], writes=[DST])
                cur, CUR = dst, DST
                sh_ *= 2
            w = 2 ** (g + 1)
            C.op("dve", lambda e, cur=cur, w=w: e.scalar_tensor_tensor(
                out=dT, in0=cur[:, :, 16:80], scalar=1.0 / w, in1=uT[:, :, 16:80], op0=ALU.mult, op1=ALU.subtract),
                reads=[CUR, UT], writes=[DT_])
            C.op("dve", lambda e, cur=cur, g=g: e.tensor_tensor(out=tmp64, in0=cur[:, 0, 16:80], in1=invc[:, g, :], op=ALU.mult),
                 reads=[CUR, KC], writes=[T64])
            C.op("dve", lambda e: e.tensor_tensor(out=dT[:, 0, :], in0=tmp64, in1=uT[:, 0, 16:80], op=ALU.subtract),
                 reads=[T64, UT], writes=[DT_])
            for tg in range(4):
                bk = 4 + (tg % 2)
                pp = bank(bk)
                C.group("pe", [lambda e, pp=pp, g=g, tg=tg: e.matmul(pp, pwb[:, g, :], dT_f[:, tg * 512:(tg + 1) * 512],
                                                                     start=True, stop=True)],
                        reads=[PW, DT_], writes=[PB[bk]])
                C.op("act", lambda e, pp=pp, g=g, tg=tg: e.activation(
                    out=pmT[:, g, tg * 512:(tg + 1) * 512], in_=pp, func=AF.Copy, scale=psc[:, g:g + 1]),
                    reads=[PB[bk], KC], writes=[PM])
        dump("qaug", qaug, QA); dump("pmT", pmT, [PM]); dump("h_own", h_own_f, HOK)
        C.barrier()
        if stop_here("B"):
            return nc
        A.release("wu", "pwb",
                  "uT", "sA", "sB", "dT", "tmp64", "xa0", "xa1", "xa2", "xs0", "xs1", "xs2", "xs3", "xs4", "xs5")

        attn_o = A.alloc("attn_o", [128, 16, 512], BF16)
        AO = Buf()
        kaug = [A.alloc(f"kaug{i}", [96, S], BF16) for i in range(2)]
        KA = [Buf() for _ in range(2)]
        vaug = [A.alloc(f"vaug{i}", [128, 64, 65], BF16) for i in range(2)]
        VA = [Buf() for _ in range(2)]
        bnear = [A.alloc(f"bnear{i}", [128, 10, 256], BF16) for i in range(2)]
        BN = [Buf() for _ in range(2)]
        pT = [A.alloc(f"pT{i}", [128, 1024], BF16) for i in range(3)]
        PT = [Buf() for _ in range(3)]
        rsc = A.alloc("rsc", [128, 8], F32)
        RS = [Buf() for _ in range(8)]
        for i in range(2):
            C.dma("pool", kaug[i][64:96, :], e_rows, writes=[KA[i]])
        v_dram3 = v_dram.rearrange("(t p) (h d) -> p t h d", p=128, h=8)
        sn = 0
        on = 0
        for h in range(NH):
            ks, KS = kaug[h % 2], KA[h % 2]
            C.dma("sp", ks[0:64, :], kt_dram[h * 64:(h + 1) * 64, :], reads=[KTD], writes=[KS])
            hp, hh = h // 2, h % 2
            va, VA_ = vaug[h % 2], VA[h % 2]
            for q4 in range(4):
                C.dma("sp", va[:, q4 * 16:(q4 + 1) * 16, :], v_dram3[:, q4 * 16:(q4 + 1) * 16, h, :],
                      reads=[VD], writes=[VA_])
            bn_, BN_ = bnear[h % 2], BN[h % 2]
            C.dma("pool", bn_, bias_near[h], writes=[BN_])
            for g in range(8):
                n0 = 4 * g - 1
                nlast = 4 * g + 3
                blocks = list(range(0, nlast + 1))
                po = [bank(6 + qt)[:, 0:65] for qt in range(2)]
                PO = [PB[6 + qt] for qt in range(2)]
                on += 1

                def emit_S(unit):
                    nonlocal sn
                    pi = sn % 3
                    sn += 1
                    ps2 = psum_t[:, 2 * pi * 512:(2 * pi + 2) * 512]
                    near = unit[0] >= n0
                    fns = []
                    for j, n in enumerate(unit):
                        for kt in range(2):
                            o_ = ps2[:, j * 512 + kt * 256:j * 512 + (kt + 1) * 256]
                            fns.append(lambda e, kt=kt, o_=o_, n=n: e.matmul(
                                o_, ks[:, n * 256 + kt * 128:n * 256 + (kt + 1) * 128],
                                qaug[:, h, g * 256:(g + 1) * 256], start=True, stop=(not near)))
                            if near:
                                fns.append(lambda e, kt=kt, o_=o_, n=n: e.matmul(
                                    o_, ident_b, bn_[:, (n - n0) * 2 + kt, :], start=False, stop=True))
                    C.group("pe", fns, reads=[KS, QA[h], KC] + ([BN_] if near else []), writes=[PB[2 * pi], PB[2 * pi + 1]])
                    w_ = 512 * len(unit)
                    C.op("act", lambda e, ps2=ps2, pi=pi, near=near, w_=w_: e.activation(
                        out=pT[pi][:, 0:w_], in_=ps2[:, 0:w_], func=AF.Exp, bias=(zero_c if near else b31[:, h:h + 1]), scale=1.0),
                        reads=[PB[2 * pi], PB[2 * pi + 1], KC], writes=[PT[pi]])
                    return pi

                def emit_PV(unit, pi):
                    pt_, PT_ = pT[pi], PT[pi]
                    fns = []
                    for j, n in enumerate(unit):
                        for qt in range(2):
                            if qt == 0 and n >= 4 * g + 2:
                                continue
                            lastn = nlast if qt == 1 else 4 * g + 1
                            for kt in range(2):
                                fns.append(lambda e, qt=qt, kt=kt, n=n, j=j, lastn=lastn: e.matmul(
                                    po[qt], pt_[:, j * 512 + kt * 256 + qt * 128:j * 512 + kt * 256 + (qt + 1) * 128],
                                    va[:, n * 2 + kt, :], start=(n == 0 and kt == 0), stop=(n == lastn and kt == 1)))
                    C.group("pe", fns, reads=[PT_, VA_], writes=[PO[0], PO[1]])

                units = []
                for lst in ([n for n in blocks if n < n0], [n for n in blocks if n >= n0]):
                    for i_ in range(0, len(lst), 2):
                        units.append(lst[i_:i_ + 2])
                pend = []
                for u_ in units:
                    pend.append((u_, emit_S(u_)))
                    if len(pend) > 2:
                        emit_PV(*pend.pop(0))
                while pend:
                    emit_PV(*pend.pop(0))
                for qt in range(2):
                    ri = (on * 2 + qt) % 8
                    C.op("dve", lambda e, qt=qt, ri=ri: e.reciprocal(out=rsc[:, ri:ri + 1], in_=po[qt][:, 64:65]),
                         reads=[PO[qt]], writes=[RS[ri]])
                    C.op("dve", lambda e, qt=qt, ri=ri: e.tensor_scalar(
                        out=attn_o[:, g * 2 + qt, h * 64:(h + 1) * 64], in0=po[qt][:, 0:64], scalar1=rsc[:, ri:ri + 1],
                        scalar2=None, op0=ALU.mult), reads=[PO[qt], RS[ri]], writes=[AO])
        dump("attn_o", attn_o, [AO])
        C.barrier()
        if stop_here("C"):
            return nc
        A.release("qaug", "kaug0", "kaug1", "vaug0", "vaug1", "bnear0", "bnear1", "pT0", "pT1", "pT2", "rsc")

        mT = A.alloc("mT", [128, 8, NOWN], BF16)
        MT = Buf()
        wba = A.alloc("wba", [128, 4, D], BF16)
        wbp = A.alloc("wbp", [128, 4, D], BF16)
        wgt = A.alloc("wgt", [128, 8, 2048], BF16)
        WBA, WBP, WGT = Buf(), Buf(), Buf()
        wload(wba, w_ba, 4, [WBA])
        wload(wbp, w_bp, 4, [WBP])
        for i in range(4):
            C.dma("pool", wgt[:, :, i * 512:(i + 1) * 512],
                  w_in[:, 2048 + i * 512:2048 + (i + 1) * 512].rearrange("(k p) n -> p k n", p=128), writes=[WGT])
        aoT = [A.alloc(f"aoT{i}", [128, 4, 512], BF16) for i in range(2)]
        AOT = [Buf() for _ in range(2)]
        sg = [A.alloc(f"sg{i}", [128, 512], F32) for i in range(4)]
        SG = [Buf() for _ in range(4)]
        m1 = [A.alloc(f"m1_{i}", [128, 512], F32) for i in range(2)]
        M1 = [Buf() for _ in range(2)]
        for tg in range(4):
            at, AT = aoT[tg % 2], AOT[tg % 2]
            for fc in range(4):
                bk = fc % 2
                ptb = bank(bk, BF16)
                C.group("pe", [(lambda e, qt=qt, fc=fc, ptb=ptb, tg=tg: e.transpose(
                    ptb[:, qt * 128:(qt + 1) * 128], attn_o[:, tg * 4 + qt, fc * 128:(fc + 1) * 128], ident_b))
                    for qt in range(4)], reads=[AO, KC], writes=[PB[bk]])
                C.op("dve", lambda e, fc=fc, ptb=ptb, at=at: e.tensor_copy(out=at[:, fc, :], in_=ptb[:, 0:512]),
                     reads=[PB[bk]], writes=[AT])
            for ft in range(8):
                i2 = (tg * 8 + ft) % 2
                pa, pga, pp, pgp = bank(2), bank(3), bank(4), bank(5)
                C.group("pe", [(lambda e, fc=fc, ft=ft: e.matmul(pa, wba[:, fc, ft * 128:(ft + 1) * 128], at[:, fc, :],
                                                                  start=(fc == 0), stop=(fc == 3))) for fc in range(4)],
                        reads=[WBA, AT], writes=[PB[2]])
                C.group("pe", [(lambda e, kc=kc, ft=ft, tg=tg: e.matmul(
                    pga, wgt[:, kc, ft * 128:(ft + 1) * 128], h_own[:, kc, tg * 8:(tg + 1) * 8, 16:80],
                    start=(kc == 0), stop=(kc == 7))) for kc in range(8)], reads=[WGT] + HOK, writes=[PB[3]])
                C.group("pe", [(lambda e, g=g, ft=ft, tg=tg: e.matmul(
                    pp, wbp[:, g, ft * 128:(ft + 1) * 128], pmT[:, g, tg * 512:(tg + 1) * 512],
                    start=(g == 0), stop=(g == 3))) for g in range(4)], reads=[WBP, PM], writes=[PB[4]])
                C.group("pe", [(lambda e, kc=kc, ft=ft, tg=tg: e.matmul(
                    pgp, wgt[:, kc, 1024 + ft * 128:1024 + (ft + 1) * 128], h_own[:, kc, tg * 8:(tg + 1) * 8, 16:80],
                    start=(kc == 0), stop=(kc == 7))) for kc in range(8)], reads=[WGT] + HOK, writes=[PB[5]])
                sa, SA2 = sg[2 * i2], SG[2 * i2]
                sp_, SP2 = sg[2 * i2 + 1], SG[2 * i2 + 1]
                C.op("act", lambda e, sa=sa, ft=ft: e.activation(out=sa, in_=pga, func=AF.Sigmoid, bias=bgc[:, ft:ft + 1], scale=1.0),
                     reads=[PB[3], KC], writes=[SA2])
                C.op("act", lambda e, sp_=sp_, ft=ft: e.activation(out=sp_, in_=pgp, func=AF.Sigmoid, bias=bgc[:, 8 + ft:9 + ft], scale=1.0),
                     reads=[PB[5], KC], writes=[SP2])
                mm, MM = m1[i2], M1[i2]
                C.op("dve", lambda e, mm=mm, sa=sa: e.tensor_tensor(out=mm, in0=pa, in1=sa, op=ALU.mult),
                     reads=[PB[2], SA2], writes=[MM])
                C.op("dve", lambda e, sp_=sp_: e.tensor_tensor(out=sp_, in0=pp, in1=sp_, op=ALU.mult),
                     reads=[PB[4], SP2], writes=[SP2])
                C.op("pool", lambda e, mm=mm, sp_=sp_, ft=ft, tg=tg: e.tensor_tensor(
                    out=mT[:, ft, tg * 512:(tg + 1) * 512], in0=mm, in1=sp_, op=ALU.add),
                    reads=[MM, SP2], writes=[MT])
        dump("mT", mT, [MT])
        C.barrier()
        if stop_here("P1"):
            return nc
        A.release("h_own", "pmT", "attn_o", "wba", "wbp", "wgt", "aoT0", "aoT1", "sg0", "sg1", "sg2", "sg3", "m1_0", "m1_1")

        h2T = A.alloc("h2T", [128, 8, NOWN], BF16)
        H2 = Buf()
        xa = [A.alloc(f"xa{i}", [128, D], F32) for i in range(3)]
        XA = [Buf() for _ in range(3)]
        wo = A.alloc("wo", [128, 8, D], BF16)
        WO = Buf()
        wload(wo, w_out, 8, [WO])
        wr_sb = A.alloc("wr_sb", [128, 8, 36], F32)
        WR = Buf()
        C.dma("sp", wr_sb, w_r.rearrange("(k p) n -> p k n", p=128), writes=[WR])
        x1t = [A.alloc(f"x1t{i}", [128, D], F32) for i in range(2)]
        X1 = [Buf() for _ in range(2)]
        xsf = [A.alloc(f"xsf{i}", [128, D], F32) for i in range(2)]
        XSF = [Buf() for _ in range(2)]
        h2f = [A.alloc(f"h2f{i}", [128, 8, 128], F32) for i in range(2)]
        H2F = [Buf() for _ in range(2)]
        zall = A.alloc("zall", [128, 16, 36], F32)
        ZA = Buf()
        rv = A.alloc("rv", [128, 5, 16], F32)
        r4a = A.alloc("r4a", [128, 16, 4], F32)
        r4b = A.alloc("r4b", [128, 16, 4], F32)
        r32a = A.alloc("r32a", [128, 16, 32], F32)
        r32b = A.alloc("r32b", [128, 16, 32], F32)
        X1D = Buf()
        wg = [A.alloc(f"wg{i}", [128, 8, 512], BF16) for i in range(2)]
        wu2 = [A.alloc(f"wu2{i}", [128, 8, 512], BF16) for i in range(2)]
        wd = [A.alloc(f"wd{i}", [128, 4, D], BF16) for i in range(2)]
        WG, WU2, WD = [Buf(), Buf()], [Buf(), Buf()], [Buf(), Buf()]
        for ex in range(2):
            wload(wg[ex], w_eg[ex], 8, [WG[ex]])
            wload(wu2[ex], w_eu[ex], 8, [WU2[ex]])
            wload(wd[ex], w_ed[ex], 4, [WD[ex]])
        def p2_X(qtile):
            i2 = qtile % 2
            xo, XO = xa[qtile % 3], XA[qtile % 3]
            for half in range(2):
                ch = 2 * qtile + half
                C.dma("sp", xo[half * 64:(half + 1) * 64, :], x_own[ch * 80 + 16:ch * 80 + 80, :], writes=[XO])
            x1, X1_ = x1t[i2], X1[i2]
            for half in range(2):
                pm_ = bank(half)
                C.group("pe", [(lambda e, ft=ft, half=half, pm_=pm_: e.matmul(
                    pm_, mT[:, ft, qtile * 128:(qtile + 1) * 128], wo[:, ft, half * 512:(half + 1) * 512],
                    start=(ft == 0), stop=(ft == 7))) for ft in range(8)], reads=[MT, WO], writes=[PB[half]])
                C.op("dve", lambda e, half=half, pm_=pm_, x1=x1: e.tensor_tensor(
                    out=x1[:, half * 512:(half + 1) * 512], in0=pm_, in1=gate1_rep[:, half * 512:(half + 1) * 512], op=ALU.mult),
                    reads=[PB[half], BM2], writes=[X1_])
            C.op("dve", lambda e, x1=x1, xo=xo: e.tensor_tensor(out=x1, in0=x1, in1=xo, op=ALU.add),
                 reads=[XO, X1_], writes=[X1_])
            C.dma("sp", x1_dram[qtile * 128:(qtile + 1) * 128, :], x1, reads=[X1_], writes=[X1D])
            s4 = stat[:, qtile % 8, :]
            SB_ = ST[qtile % 8]
            C.op("act", lambda e, x1=x1, s4=s4: e.activation(out=junk, in_=x1, func=AF.Square, accum_out=s4[:, 0:1]),
                 reads=[X1_], writes=[SB_])
            C.op("act", lambda e, s4=s4: e.activation(out=s4[:, 1:2], in_=s4[:, 0:1], func=AF.Sqrt, scale=1.0 / D, bias=eps_c),
                 reads=[SB_, KC], writes=[SB_])
            C.op("dve", lambda e, s4=s4: e.reciprocal(out=s4[:, 2:3], in_=s4[:, 1:2]), reads=[SB_], writes=[SB_])
            xf, XF = xsf[i2], XSF[i2]
            C.op("dve", lambda e, xf=xf, x1=x1, s4=s4: e.tensor_scalar(out=xf, in0=x1, scalar1=s4[:, 2:3], scalar2=None, op0=ALU.mult),
                 reads=[X1_, SB_], writes=[XF])

        def p2_Y(qtile):
            i2 = qtile % 2
            xf, XF = xsf[i2], XSF[i2]
            hf, HF = h2f[i2], H2F[i2]
            for hb in range(2):
                bk = 2 + hb
                ptf = bank(bk).rearrange("p (a b) -> p a b", a=4)
                C.group("pe", [(lambda e, k4=k4, hb=hb, ptf=ptf, xf=xf: e.transpose(
                    ptf[:, k4, :], xf[:, (hb * 4 + k4) * 128:(hb * 4 + k4 + 1) * 128], ident_f)) for k4 in range(4)],
                    reads=[XF, KC], writes=[PB[bk]])
                C.op("dve", lambda e, hb=hb, ptf=ptf, hf=hf: e.tensor_tensor(
                    out=hf[:, hb * 4:(hb + 1) * 4, :], in0=ptf, in1=gm2[:, hb * 4:(hb + 1) * 4].unsqueeze(2).to_broadcast([128, 4, 128]),
                    op=ALU.mult), reads=[PB[bk], BM2], writes=[HF])
            C.op("dve", lambda e, hf=hf: e.tensor_tensor(out=hf, in0=hf, in1=sh2.unsqueeze(2).to_broadcast([128, 8, 128]), op=ALU.add),
                 reads=[HF, BM2], writes=[HF])
            C.op("act", lambda e, hf=hf: e.activation(out=h2T[:, :, qtile * 128:(qtile + 1) * 128], in_=hf, func=AF.Copy),
                 reads=[HF], writes=[H2])
            pz = bank(4 + i2)
            C.group("pe", [(lambda e, kc=kc, pz=pz, hf=hf: e.matmul(pz[:, 0:36], hf[:, kc, :], wr_sb[:, kc, :],
                                                                     start=(kc == 0), stop=(kc == 7))) for kc in range(8)],
                    reads=[HF, WR], writes=[PB[4 + i2]])
            C.op("dve", lambda e, pz=pz: e.tensor_tensor(out=zall[:, qtile, :], in0=pz[:, 0:36], in1=b_r_rep, op=ALU.add),
                 reads=[PB[4 + i2], KC], writes=[ZA])
        p2_X(0)
        for qtile in range(16):
            if qtile + 1 < 16:
                p2_X(qtile + 1)
            p2_Y(qtile)
        zg = zall[:, :, 0:4]
        ze = zall[:, :, 4:36]
        bc4 = lambda v: v.unsqueeze(2).to_broadcast([128, 16, 4])
        bc32 = lambda v: v.unsqueeze(2).to_broadcast([128, 16, 32])
        RB = Buf()
        C.op("dve", lambda e: e.tensor_reduce(out=rv[:, 0, :], in_=zg, axis=AX.X, op=ALU.max), reads=[ZA], writes=[RB])
        C.op("dve", lambda e: e.tensor_tensor(out=r4a, in0=zg, in1=bc4(rv[:, 0, :]), op=ALU.subtract), reads=[ZA, RB], writes=[RB])
        C.op("act", lambda e: e.activation(out=r4b, in_=r4a, func=AF.Exp), reads=[RB], writes=[RB])
        C.op("dve", lambda e: e.tensor_reduce(out=rv[:, 1, :], in_=r4b, axis=AX.X, op=ALU.add), reads=[RB], writes=[RB])
        C.op("dve", lambda e: e.reciprocal(out=rv[:, 1, :], in_=rv[:, 1, :]), reads=[RB], writes=[RB])
        C.op("dve", lambda e: e.tensor_scalar(out=r4a, in0=r4a, scalar1=0.0, scalar2=1e30, op0=ALU.is_lt, op1=ALU.mult),
             reads=[RB], writes=[RB])
        C.op("dve", lambda e: e.tensor_tensor(
            out=r32a.rearrange("p t (a b) -> p t a b", a=4), in0=ze.rearrange("p t (a b) -> p t a b", a=4),
            in1=r4a.unsqueeze(3).to_broadcast([128, 16, 4, 8]), op=ALU.subtract), reads=[ZA, RB], writes=[RB])
        C.op("dve", lambda e: e.tensor_reduce(out=rv[:, 2, :], in_=r32a, axis=AX.X, op=ALU.max), reads=[RB], writes=[RB])
        C.op("dve", lambda e: e.tensor_tensor(out=r32b, in0=r32a, in1=bc32(rv[:, 2, :]), op=ALU.is_ge), reads=[RB], writes=[RB])
        C.op("dve", lambda e: e.scalar_tensor_tensor(out=r32b, in0=r32b, scalar=-1e30, in1=r32a, op0=ALU.mult, op1=ALU.add),
             reads=[RB], writes=[RB])
        C.op("dve", lambda e: e.tensor_reduce(out=rv[:, 3, :], in_=r32b, axis=AX.X, op=ALU.max), reads=[RB], writes=[RB])
        C.op("dve", lambda e: e.tensor_tensor(out=r32b, in0=r32a, in1=bc32(rv[:, 3, :]), op=ALU.is_ge), reads=[RB], writes=[RB])
        C.op("dve", lambda e: e.tensor_tensor(out=r32a, in0=r32a, in1=bc32(rv[:, 2, :]), op=ALU.subtract), reads=[RB], writes=[RB])
        C.op("act", lambda e: e.activation(out=r32a, in_=r32a, func=AF.Exp), reads=[RB], writes=[RB])
        C.op("dve", lambda e: e.tensor_tensor(out=r32a, in0=r32a, in1=r32b, op=ALU.mult), reads=[RB], writes=[RB])
        C.op("dve", lambda e: e.tensor_reduce(out=rv[:, 4, :], in_=r32a, axis=AX.X, op=ALU.add), reads=[RB], writes=[RB])
        C.op("dve", lambda e: e.reciprocal(out=rv[:, 4, :], in_=rv[:, 4, :]), reads=[RB], writes=[RB])
        C.op("dve", lambda e: e.tensor_tensor(out=rv[:, 4, :], in0=rv[:, 4, :], in1=rv[:, 1, :], op=ALU.mult), reads=[RB], writes=[RB])
        C.op("dve", lambda e: e.tensor_tensor(out=comb, in0=r32a, in1=bc32(rv[:, 4, :]), op=ALU.mult), reads=[RB], writes=[CB])
        dump("h2T", h2T, [H2]); dump("comb", comb, [CB])
        C.barrier()
        if stop_here("P2"):
            return nc
        A.release("mT", "wo", "wr_sb", "x1t0", "x1t1", "xsf0", "xsf1", "h2f0", "h2f1", "zall", "rv", "r4a", "r4b", "r32a", "r32b")

        yq = [A.alloc(f"yacc{i}", [128, 4, D], F32) for i in range(4)]
        YA = [Buf() for _ in range(16)]
        aT = [A.alloc(f"aT{i}", [128, 4, 512], BF16) for i in range(2)]
        AT2 = [Buf() for _ in range(2)]
        sgl = [A.alloc(f"sgl{i}", [128, 512], F32) for i in range(2)]
        SGL = [Buf() for _ in range(2)]
        for i in range(4):
            C.op("pool", lambda e, i=i: e.memset(yq[i].rearrange("p a b -> p (a b)"), 0.0), writes=YA[4 * i:4 * i + 4])
        def final_tile(tile):
            xo, XO = xa[tile % 3], XA[tile % 3]
            C.dma("sp", xo, x1_dram[tile * 128:(tile + 1) * 128, :], reads=[X1D], writes=[XO])
            yt = yq[tile // 4][:, tile % 4, :]
            C.op("dve", lambda e, yt=yt: e.tensor_tensor(out=yt, in0=yt, in1=gate2_rep, op=ALU.mult),
                 reads=[YA[tile], BM2], writes=[YA[tile]])
            C.op("pool", lambda e, yt=yt, xo=xo: e.tensor_tensor(out=yt, in0=yt, in1=xo, op=ALU.add),
                 reads=[YA[tile], XO], writes=[YA[tile]])
            s4 = stat[:, tile % 8, :]
            SB_ = ST[tile % 8]
            C.op("act", lambda e, yt=yt, s4=s4: e.activation(out=junk, in_=yt, func=AF.Square, accum_out=s4[:, 0:1]),
                 reads=[YA[tile]], writes=[SB_])
            C.op("act", lambda e, s4=s4: e.activation(out=s4[:, 1:2], in_=s4[:, 0:1], func=AF.Sqrt, scale=1.0 / D, bias=eps_c),
                 reads=[SB_, KC], writes=[SB_])
            C.op("dve", lambda e, s4=s4: e.reciprocal(out=s4[:, 2:3], in_=s4[:, 1:2]), reads=[SB_], writes=[SB_])
            C.op("dve", lambda e, yt=yt, s4=s4: e.scalar_tensor_tensor(
                out=yt, in0=yt, scalar=s4[:, 2:3], in1=gf_rep, op0=ALU.mult, op1=ALU.mult),
                reads=[YA[tile], SB_, KC], writes=[YA[tile]])
            C.dma("sp", out_d[tile * 128:(tile + 1) * 128, :], yt, reads=[YA[tile]], is_output=True)

        an = 0
        sn2 = 0
        yn = 0
        for ex in range(NEXP):
            s_ = ex % 2
            if ex >= 2:
                wload(wg[s_], w_eg[ex], 8, [WG[s_]])
                wload(wu2[s_], w_eu[ex], 8, [WU2[s_]])
                wload(wd[s_], w_ed[ex], 4, [WD[s_]])
            for tg in range(4):
                at, AT_ = aT[an % 2], AT2[an % 2]
                an += 1
                for f in range(4):
                    bg_, bu_ = 0 + (sn2 % 2), 2 + (sn2 % 2)
                    pg_, pu_ = bank(bg_), bank(bu_)
                    sl, SL = sgl[sn2 % 2], SGL[sn2 % 2]
                    sn2 += 1
                    C.group("pe", [(lambda e, kc=kc, f=f, tg=tg, pg_=pg_: e.matmul(
                        pg_, wg[s_][:, kc, f * 128:(f + 1) * 128], h2T[:, kc, tg * 512:(tg + 1) * 512],
                        start=(kc == 0), stop=(kc == 7))) for kc in range(8)], reads=[WG[s_], H2], writes=[PB[bg_]])
                    C.group("pe", [(lambda e, kc=kc, f=f, tg=tg, pu_=pu_: e.matmul(
                        pu_, wu2[s_][:, kc, f * 128:(f + 1) * 128], h2T[:, kc, tg * 512:(tg + 1) * 512],
                        start=(kc == 0), stop=(kc == 7))) for kc in range(8)], reads=[WU2[s_], H2], writes=[PB[bu_]])
                    C.op("act", lambda e, sl=sl, pg_=pg_: e.activation(out=sl, in_=pg_, func=AF.Silu), reads=[PB[bg_]], writes=[SL])
                    C.op("dve", lambda e, sl=sl, pu_=pu_, at=at, f=f: e.tensor_tensor(out=at[:, f, :], in0=pu_, in1=sl, op=ALU.mult),
                         reads=[PB[bu_], SL], writes=[AT_])
                for t in range(4):
                    tile = tg * 4 + t
                    for half in range(2):
                        by = 4 + (yn % 4)
                        yn += 1
                        py = bank(by)
                        C.group("pe", [(lambda e, fc=fc, t=t, half=half, py=py, at=at: e.matmul(
                            py, at[:, fc, t * 128:(t + 1) * 128], wd[s_][:, fc, half * 512:(half + 1) * 512],
                            start=(fc == 0), stop=(fc == 3))) for fc in range(4)], reads=[AT_, WD[s_]], writes=[PB[by]])
                        C.op("dve", lambda e, py=py, tile=tile, half=half, ex=ex: e.scalar_tensor_tensor(
                            out=yq[tile // 4][:, tile % 4, half * 512:(half + 1) * 512], in0=py, scalar=comb[:, tile, ex:ex + 1],
                            in1=yq[tile // 4][:, tile % 4, half * 512:(half + 1) * 512], op0=ALU.mult, op1=ALU.add),
                            reads=[PB[by], CB, YA[tile]], writes=[YA[tile]])
                    if ex == NEXP - 1 and half == 1:
                        final_tile(tile)
        C.finish()
    return nc


def t5_bucket_np(rel):
    n = np.maximum(rel, 0)
    nf = np.maximum(n, 1).astype(np.float32)
    large = 16 + (np.log(nf / 16) / math.log(128 / 16) * 16).astype(np.int32)
    large = np.minimum(large, 31)
    return np.where(n < 16, n, large)


def host_inputs(inp, c):
    b, j = c // 4, c % 4
    f = np.float32
    x = inp["x"]
    xb = np.ascontiguousarray(x[b])
    pos = (np.arange(32)[:, None] * 256 + 64 * j - 16 + np.arange(80)[None, :]).reshape(-1)
    x_own = np.zeros((2560, D), f)
    ok = pos >= 0
    x_own[ok] = xb[pos[ok]]
    col = lambda v: np.ascontiguousarray(np.asarray(v, f).reshape(-1, 128).T)
    rel_bias = np.asarray(inp["rel_bias"], f)
    rel = np.arange(5)[:, None, None, None, None] - 1
    kt = np.arange(2)[None, :, None, None, None]
    kp = np.arange(128)[None, None, :, None, None]
    cq = np.arange(4)[None, None, None, :, None]
    r = np.arange(64)[None, None, None, None, :]
    d = (cq - rel) * 256 + 64 * j + r - kt * 128 - kp
    bucket = t5_bucket_np(d)
    bn = np.empty((NH, 128, 10, 256), f)
    for h in range(NH):
        tb = np.where(d >= 0, rel_bias[bucket, h], f(NEG))
        bn[h] = tb.transpose(2, 0, 1, 3, 4).reshape(128, 10, 256)
    e_rows = (np.arange(S)[None, :] // 256 == np.arange(32)[:, None]).astype(f)
    iq = (2 * np.arange(16)[None, :] + (np.arange(128)[:, None] // 64))
    nn = np.arange(32)[None, None, :]
    negmask = np.where(nn >= iq[:, :, None], f(-1e30), f(0)).astype(f)
    valid01 = (nn < iq[:, :, None]).astype(f)
    ownm1 = (nn == iq[:, :, None]).astype(f) - 1.0
    halo = np.full((128, 16), 0.0 if j == 0 else 1.0, f)
    t = 64 * j + np.arange(64)
    invc = np.stack([1.0 / np.minimum(t + 1, w) for w in (2, 4, 8, 16)], 0).astype(f)
    invc = np.broadcast_to(invc[None], (128, 4, 64)).copy()
    m = {
        "x_all": xb, "x_own": x_own,
        "w_in": np.ascontiguousarray(inp["w_in"][0]), "w_ada": np.ascontiguousarray(inp["w_ada"][0]),
        "b_ada": np.ascontiguousarray(inp["b_ada"][0][None, :]),
        "ccol": col(inp["c"][b]), "g1col": col(inp["norm1_g"][0]), "g2col": col(inp["norm2_g"][0]),
        "gf_rep": np.broadcast_to(np.asarray(inp["norm_f_g"], f)[None, :], (128, D)).copy(),
        "bgate_col": col(inp["b_gate"][0]), "pscale_col": col(inp["pool_scale"][0]),
        "b31_rep": np.broadcast_to(rel_bias[31][None, :], (128, 8)).copy(),
        "pool_w": np.ascontiguousarray(inp["pool_w"][0]),
        "w_ba": np.ascontiguousarray(inp["w_branch_attn"][0]), "w_bp": np.ascontiguousarray(inp["w_branch_pool"][0]),
        "w_out": np.ascontiguousarray(inp["w_out"][0]),
        "w_r": np.ascontiguousarray(np.concatenate([inp["w_router_group"][0], inp["w_router_expert"][0]], axis=1)),
        "b_r_rep": np.broadcast_to(np.concatenate([inp["b_router_group"][0], inp["b_router_expert"][0]])[None, :],
                                   (128, 36)).astype(f).copy(),
        "w_eg": np.ascontiguousarray(inp["w_expert_gate"][0]), "w_eu": np.ascontiguousarray(inp["w_expert_up"][0]),
        "w_ed": np.ascontiguousarray(inp["w_expert_down"][0]),
        "bias_near": bn, "e_rows": e_rows, "negmask": negmask, "valid01": valid01, "ownm1": ownm1.astype(f),
        "halo_mask": halo, "invcnt0": invc, "ident": np.eye(128, dtype=f),
    }
    return {k: np.ascontiguousarray(np.asarray(v, f)) for k, v in m.items()}


def kernel(**inputs):
    inp = {k: np.asarray(v) for k, v in inputs.items()}
    nc = build_program()
    in_maps = [host_inputs(inp, c) for c in range(8)]
    res = run_bass_kernel_spmd(nc, in_maps, core_ids=list(range(8)))
    out = np.empty((2, S, D), np.float32)
    for c in range(8):
        b, j = c // 4, c % 4
        o = np.asarray(res.results[c]["out"]).reshape(32, 64, D)
        out[b].reshape(32, 256, D)[:, 64 * j:64 * j + 64, :] = o
    return out
```
